# Optimizing a Trainium2 kernel written in Bass

```python
import math
import jax, jax.numpy as jnp
from jax import lax
import numpy as np

D_MODEL = 1024
BATCH = 8
SEQ = 4096
DEPTH = 1

CHUNK = 64
Q_BLOCK = 128
HEAD_DIM = 64
FOX_HEADS = 8
FOX_WIDTH = FOX_HEADS * HEAD_DIM
DIFF_HEADS = 4
DIFF_V_DIM = 2 * HEAD_DIM
DIFF_QK_WIDTH = DIFF_HEADS * 2 * HEAD_DIM
DIFF_WIDTH = DIFF_HEADS * DIFF_V_DIM
MIX_WIDTH = FOX_WIDTH + DIFF_WIDTH
IN_COLS = 3 * FOX_WIDTH + FOX_HEADS + 2 * DIFF_QK_WIDTH + DIFF_WIDTH
N_GROUPS = 4
EXPERTS_PER_GROUP = 8
N_EXPERTS = N_GROUPS * EXPERTS_PER_GROUP
TOP_K_IN_GROUP = 2
D_EXPERT = 512
DISPATCH_BLOCK = 256
NORM_EPS = 1e-6
SUBLN_EPS = 1e-5
FORGET_BIAS_MEAN = 3.0

kernel_name = 'hybrid_fox_diffattn_hmoe_adaln'


def rms_norm(x, g, eps=NORM_EPS):
    xf = x.astype(jnp.float32)
    y = xf * lax.rsqrt(jnp.mean(xf * xf, axis=-1, keepdims=True) + eps)
    return (y * g.astype(jnp.float32)).astype(x.dtype)


def alibi_slopes(n):
    return jnp.asarray([2.0 ** (-8.0 * (i + 1) / n) for i in range(n)], dtype=jnp.float32)


def forgetting_attention(q, k, v, log_f_cum):
    seq = q.shape[1]
    scale = HEAD_DIM ** -0.5
    cum = jnp.swapaxes(log_f_cum, 1, 2)
    outs = []
    for q0 in range(0, seq, Q_BLOCK):
        q1 = q0 + Q_BLOCK
        s = jnp.einsum('bqhd,bkhd->bhqk', q[:, q0:q1], k[:, :q1],
                       preferred_element_type=jnp.float32) * scale
        s = s + (cum[:, :, q0:q1, None] - cum[:, :, None, :q1])
        causal = jnp.arange(q0, q1)[:, None] >= jnp.arange(q1)[None, :]
        p = jax.nn.softmax(jnp.where(causal, s, -jnp.inf), axis=-1)
        outs.append(jnp.einsum('bhqk,bkhd->bqhd', p.astype(v.dtype), v[:, :q1]))
    return jnp.concatenate(outs, axis=1)


def differential_attention(q, k, v, lam):
    seq = q.shape[1]
    scale = HEAD_DIM ** -0.5
    slopes = alibi_slopes(DIFF_HEADS)
    outs = []
    for q0 in range(0, seq, Q_BLOCK):
        q1 = q0 + Q_BLOCK
        tq = jnp.arange(q0, q1)
        tk = jnp.arange(q1)
        s = jnp.einsum('bqhmd,bkhmd->bhmqk', q[:, q0:q1], k[:, :q1],
                       preferred_element_type=jnp.float32) * scale
        dist = jnp.abs(tq[:, None] - tk[None, :]).astype(jnp.float32)
        s = s - slopes[:, None, None, None] * dist
        chunk_ok = (tq // CHUNK)[:, None] >= (tk // CHUNK)[None, :]
        p = jax.nn.softmax(jnp.where(chunk_ok, s, -jnp.inf), axis=-1)
        a = p[:, :, 0] - lam * p[:, :, 1]
        outs.append(jnp.einsum('bhqk,bkhe->bqhe', a.astype(v.dtype), v[:, :q1]))
    return jnp.concatenate(outs, axis=1)


def hybrid_mixer(h, w_in, b_f, lam_q1, lam_k1, lam_q2, lam_k2, subln_g, w_o, lam_init):
    b, s, _ = h.shape
    proj = h @ w_in
    cuts = [FOX_WIDTH, 2 * FOX_WIDTH, 3 * FOX_WIDTH, 3 * FOX_WIDTH + FOX_HEADS,
            3 * FOX_WIDTH + FOX_HEADS + DIFF_QK_WIDTH,
            3 * FOX_WIDTH + FOX_HEADS + 2 * DIFF_QK_WIDTH]
    fq, fk, fv, fz, dq, dk, dv = jnp.split(proj, cuts, axis=-1)
    log_f_cum = jnp.cumsum(jax.nn.log_sigmoid((fz + b_f).astype(jnp.float32)), axis=1)
    fox = forgetting_attention(fq.reshape(b, s, FOX_HEADS, HEAD_DIM),
                               fk.reshape(b, s, FOX_HEADS, HEAD_DIM),
                               fv.reshape(b, s, FOX_HEADS, HEAD_DIM), log_f_cum)
    fox = fox.reshape(b, s, FOX_WIDTH)
    f32 = jnp.float32
    lam = (jnp.exp(jnp.sum(lam_q1.astype(f32) * lam_k1.astype(f32)))
           - jnp.exp(jnp.sum(lam_q2.astype(f32) * lam_k2.astype(f32))) + lam_init)
    diff = differential_attention(dq.reshape(b, s, DIFF_HEADS, 2, HEAD_DIM),
                                  dk.reshape(b, s, DIFF_HEADS, 2, HEAD_DIM),
                                  dv.reshape(b, s, DIFF_HEADS, DIFF_V_DIM), lam)
    diff = (rms_norm(diff, subln_g, SUBLN_EPS) * (1.0 - lam_init)).reshape(b, s, DIFF_WIDTH)
    return jnp.concatenate([fox, diff], axis=-1) @ w_o


def hierarchical_route(h, w_rg, b_rg, w_re, b_re):
    t = h.shape[0]
    hf = h.astype(jnp.float32)
    g_logits = hf @ w_rg.astype(jnp.float32) + b_rg.astype(jnp.float32)
    g_prob = jax.nn.softmax(g_logits, axis=-1)
    g = jnp.argmax(g_logits, axis=-1).astype(jnp.int32)
    p_g = jnp.take_along_axis(g_prob, g[:, None], axis=-1)
    e_logits = (hf @ w_re.astype(jnp.float32) + b_re.astype(jnp.float32)).reshape(
        t, N_GROUPS, EXPERTS_PER_GROUP)
    e_logits = jnp.take_along_axis(e_logits, g[:, None, None], axis=1)[:, 0]
    top_v, top_i = lax.top_k(e_logits, TOP_K_IN_GROUP)
    w = jax.nn.softmax(top_v, axis=-1) * p_g
    eid = g[:, None] * EXPERTS_PER_GROUP + top_i.astype(jnp.int32)
    tok = jnp.repeat(jnp.arange(t, dtype=jnp.int32), TOP_K_IN_GROUP)
    return eid.reshape(-1), tok, w.reshape(-1)


def sparse_experts(h, eid, tok, wt, w_gate, w_up, w_down):
    n_assign = eid.shape[0]
    order = jnp.argsort(eid)
    se = eid[order]
    counts = jnp.zeros((N_EXPERTS,), jnp.int32).at[eid].add(1)
    starts = jnp.cumsum(counts) - counts
    padded = (counts + DISPATCH_BLOCK - 1) // DISPATCH_BLOCK * DISPATCH_BLOCK
    pends = jnp.cumsum(padded)
    pstarts = pends - padded
    dest = pstarts[se] + jnp.arange(n_assign, dtype=jnp.int32) - starts[se]
    n_blocks = -(-n_assign // DISPATCH_BLOCK) + N_EXPERTS
    n_slots = n_blocks * DISPATCH_BLOCK
    slot_tok = jnp.zeros((n_slots,), jnp.int32).at[dest].set(tok[order])
    slot_w = jnp.zeros((n_slots,), h.dtype).at[dest].set(wt[order].astype(h.dtype))
    block_e = jnp.clip(jnp.searchsorted(pends, jnp.arange(n_blocks) * DISPATCH_BLOCK, side='right'),
                       0, N_EXPERTS - 1).astype(jnp.int32)

    def run_block(args):
        btok, bw, e = args
        xb = h[btok]
        hid = jax.nn.silu(xb @ w_gate[e]) * (xb @ w_up[e])
        return (hid @ w_down[e]) * bw[:, None]

    y = lax.map(run_block, (slot_tok.reshape(n_blocks, DISPATCH_BLOCK),
                            slot_w.reshape(n_blocks, DISPATCH_BLOCK), block_e))
    return jnp.zeros_like(h).at[slot_tok].add(y.reshape(n_slots, h.shape[-1]))


def setup_inputs(seed: int = 0) -> dict:
    key = jax.random.key(seed)
    ks = jax.random.split(key, 24)
    f32 = jnp.float32

    def nrm(k, shape, std):
        return jax.random.normal(k, shape, f32) * std

    d, L = D_MODEL, DEPTH
    return {
        'x': nrm(ks[0], (BATCH, SEQ, d), 1.0),
        'c': nrm(ks[1], (BATCH, d), 1.0),
        'ada_w': nrm(ks[2], (L, d, 6 * d), 0.5 * d ** -0.5),
        'ada_b': nrm(ks[3], (L, 6 * d), 0.02),
        'norm1_g': 1.0 + nrm(ks[4], (L, d), 0.1),
        'w_in': nrm(ks[5], (L, d, IN_COLS), d ** -0.5),
        'b_f': FORGET_BIAS_MEAN + nrm(ks[6], (L, FOX_HEADS), 0.5),
        'lam_q1': nrm(ks[7], (L, HEAD_DIM), 0.1),
        'lam_k1': nrm(ks[8], (L, HEAD_DIM), 0.1),
        'lam_q2': nrm(ks[9], (L, HEAD_DIM), 0.1),
        'lam_k2': nrm(ks[10], (L, HEAD_DIM), 0.1),
        'subln_g': 1.0 + nrm(ks[11], (L, DIFF_V_DIM), 0.1),
        'w_o': nrm(ks[12], (L, MIX_WIDTH, d), MIX_WIDTH ** -0.5),
        'norm2_g': 1.0 + nrm(ks[13], (L, d), 0.1),
        'w_rg': nrm(ks[14], (L, d, N_GROUPS), d ** -0.5),
        'b_rg': nrm(ks[15], (L, N_GROUPS), 0.01),
        'w_re': nrm(ks[16], (L, d, N_EXPERTS), d ** -0.5),
        'b_re': nrm(ks[17], (L, N_EXPERTS), 0.01),
        'w_gate': nrm(ks[18], (L, N_EXPERTS, d, D_EXPERT), d ** -0.5),
        'w_up': nrm(ks[19], (L, N_EXPERTS, d, D_EXPERT), d ** -0.5),
        'w_down': nrm(ks[20], (L, N_EXPERTS, D_EXPERT, d), D_EXPERT ** -0.5),
        'norm_f_g': 1.0 + nrm(ks[21], (d,), 0.1),
    }


def reference(x, c, ada_w, ada_b, norm1_g, w_in, b_f, lam_q1, lam_k1, lam_q2, lam_k2,
              subln_g, w_o, norm2_g, w_rg, b_rg, w_re, b_re, w_gate, w_up, w_down, norm_f_g):
    b, s, d = x.shape
    for l in range(DEPTH):
        lam_init = 0.8 - 0.6 * math.exp(-0.3 * l)
        mod = (c @ ada_w[l] + ada_b[l]).reshape(b, 6, d)
        shift1, scale1, gate1, shift2, scale2, gate2 = [mod[:, i, None, :] for i in range(6)]
        h = rms_norm(x, norm1_g[l]) * (1.0 + scale1) + shift1
        x = x + gate1 * hybrid_mixer(h, w_in[l], b_f[l], lam_q1[l], lam_k1[l], lam_q2[l],
                                     lam_k2[l], subln_g[l], w_o[l], lam_init)
        h = (rms_norm(x, norm2_g[l]) * (1.0 + scale2) + shift2).reshape(b * s, d)
        eid, tok, wt = hierarchical_route(h, w_rg[l], b_rg[l], w_re[l], b_re[l])
        moe = sparse_experts(h, eid, tok, wt, w_gate[l], w_up[l], w_down[l])
        x = x + gate2 * moe.reshape(b, s, d)
    return rms_norm(x, norm_f_g)
```

```python
import numpy as np
from contextlib import ExitStack
import concourse.bass as bass
import concourse.mybir as mybir
from concourse.bass_utils import run_bass_kernel_spmd

F32 = mybir.dt.float32
BF16 = mybir.dt.bfloat16
U32 = mybir.dt.uint32
I32 = mybir.dt.int32
AF = mybir.ActivationFunctionType
ALU = mybir.AluOpType
AX = mybir.AxisListType

S = 4096
D = 1024
NT = 32
NEG = -30000.0
BLK = 128
NB = 96
NSLOT = NB * BLK
LAM_INIT = 0.8 - 0.6
DEBUG = False
STOP = 0
NBRUN = NB
GBAR = False
NSTR = 4


class _Stop(Exception):
    pass


class Sched:
    def __init__(self, nc, es, nd=12):
        self.nc = nc
        self.streams = {k: [] for k in ("pe", "act", "dve", "pool", "sp")}
        self.csem = {k: es.enter_context(nc.semaphore("c_" + k)) for k in ("pe", "act", "dve", "pool")}
        self.ccnt = {k: 0 for k in self.csem}
        self.nd = {"sp": nd, "pool": 12}
        self.dsem = {q: [es.enter_context(nc.semaphore("d_%s%d" % (q, i))) for i in range(self.nd[q])] for q in ("sp", "pool")}
        self.dcnt = {q: [0] * self.nd[q] for q in ("sp", "pool")}
        self.drr = {"sp": 0, "pool": 0}
        self.waited = {k: {} for k in self.streams}
        self.lastw = {}
        self.readers = {}
        self.out_tokens = []

    def _deps(self, reads, writes):
        deps = []
        for r in reads:
            t = self.lastw.get(r)
            if t is not None:
                deps.append(t)
        for w in writes:
            t = self.lastw.get(w)
            if t is not None:
                deps.append(t)
            deps.extend(self.readers.get(w, {}).values())
        return deps

    def _emit_waits(self, eng, deps):
        for (key, sem, val, src) in deps:
            if src == eng and eng == "pe":
                continue
            if self.waited[eng].get(key, 0) >= val:
                continue
            self.waited[eng][key] = val
            self.streams[eng].append(lambda e, sem=sem, val=val: e.wait_ge(sem, val))

    def _record(self, tok, reads, writes):
        for w in writes:
            self.lastw[w] = tok
            self.readers[w] = {}
        for r in reads:
            d = self.readers.setdefault(r, {})
            old = d.get(tok[0])
            if old is None or old[2] < tok[2]:
                d[tok[0]] = tok

    def op(self, eng, fn, reads=(), writes=()):
        deps = self._deps(reads, writes)
        self._emit_waits(eng, deps)
        self.ccnt[eng] += 1
        val = self.ccnt[eng]
        sem = self.csem[eng]
        self.streams[eng].append(lambda e, fn=fn, sem=sem: fn(e).then_inc(sem, 1))
        tok = (eng, sem, val, eng)
        self._record(tok, reads, writes)
        return tok

    def dma(self, q, fn, reads=(), writes=(), is_out=False):
        deps = self._deps(reads, writes)
        i = self.drr[q]
        self.drr[q] = (i + 1) % self.nd[q]
        sem = self.dsem[q][i]
        prev = self.dcnt[q][i]
        key = ("d", q, i)
        if prev > 0:
            deps.append((key, sem, prev, "dma"))
        self._emit_waits(q, deps)
        self.dcnt[q][i] = prev + 16
        self.streams[q].append(lambda e, fn=fn, sem=sem: fn(e).then_inc(sem, 16))
        tok = (key, sem, prev + 16, "dma")
        self._record(tok, reads, writes)
        if is_out:
            self.out_tokens.append(tok)
        return tok

    def global_barrier(self):
        toks = []
        for k in self.csem:
            if self.ccnt[k] > 0:
                toks.append((k, self.csem[k], self.ccnt[k], "all"))
        for q in ("sp", "pool"):
            for i in range(self.nd[q]):
                if self.dcnt[q][i] > 0:
                    toks.append((("d", q, i), self.dsem[q][i], self.dcnt[q][i], "dma"))
        for eng in self.streams:
            self._emit_waits(eng, toks)
        self.lastw = {}
        self.readers = {}

    def finish(self, block):
        self._emit_waits("sp", list(self.out_tokens))
        st = self.streams

        @block.tensor
        def _(e):
            for f in st["pe"]:
                f(e)

        @block.scalar
        def _(e):
            for f in st["act"]:
                f(e)

        @block.vector
        def _(e):
            for f in st["dve"]:
                f(e)

        @block.gpsimd
        def _(e):
            for f in st["pool"]:
                f(e)

        @block.sync
        def _(e):
            for f in st["sp"]:
                f(e)


def build_nc():
    nc = bass.Bass("TRN2", target_bir_lowering=False)

    def din(name, shape, dt=F32):
        return nc.dram_tensor(name, list(shape), dt, kind="ExternalInput").ap()

    x = din("x", [S, D])
    crep = din("crep", [128, 8, 128])
    ada_w = din("ada_w", [D, 6 * D])
    adab = din("adab", [128, 6 * D])
    gvec = din("gvec", [128, 3, D])
    w_in = din("w_in", [D, 3080])
    negbf = din("negbf", [8, 1])
    lamv = din("lamv", [128, 4, 64])
    subg = din("subg", [128, 128])
    w_o = din("w_o", [D, D])
    w_r = din("w_r", [D, 36])
    b_r = din("b_r", [128, 36])
    w_gate = din("w_gate", [32 * 1024, 512])
    w_up = din("w_up", [32 * 1024, 512])
    w_down = din("w_down", [32 * 512, 1024])
    ident_d = din("ident", [128, 128])
    fmask_d = din("fmask", [128, 128])
    dbias_d = din("dbias", [128, 4, 128])
    qaug_d = din("qaug", [4, 4, S])
    kaug_d = din("kaug", [4, 4, S])
    stri_d = din("stri", [128, 128])
    pcol_d = din("pcol", [128, 1])
    jgrid_d = din("jgrid", [128, NB])
    out = nc.dram_tensor("out", [S, D], F32, kind="ExternalOutput").ap()
    dbg = {}
    if DEBUG:
        dbg["mix"] = nc.dram_tensor("dbg_mix", [S, D], F32, kind="ExternalOutput").ap()
        dbg["x1"] = nc.dram_tensor("dbg_x1", [S, D], F32, kind="ExternalOutput").ap()
        dbg["rt"] = nc.dram_tensor("dbg_rt", [128, NT, 4], F32, kind="ExternalOutput").ap()
        dbg["cum"] = nc.dram_tensor("dbg_cum", [8, S], F32, kind="ExternalOutput").ap()
    cumparts = nc.dram_tensor("cumparts", [8, 3, S], BF16, kind="Internal").ap()
    xs = nc.dram_tensor("xs", [NSLOT, D], BF16, kind="Internal").ap()
    ys = nc.dram_tensor("ys", [NSLOT, D], BF16, kind="Internal").ap()
    x1s = nc.dram_tensor("x1s", [S, D], F32, kind="Internal").ap()
    wgb = nc.dram_tensor("wgb", [32 * 128, 8 * 512], BF16, kind="Internal").ap()
    wub = nc.dram_tensor("wub", [32 * 128, 8 * 512], BF16, kind="Internal").ap()
    wdb = nc.dram_tensor("wdb", [32 * 128, 4 * 1024], BF16, kind="Internal").ap()

    with ExitStack() as es:
        def sb(name, shape, dt):
            return es.enter_context(nc.sbuf_tensor("s_" + name, list(shape), dt))

        def ps(name, shape, dt=F32):
            return es.enter_context(nc.psum_tensor("p_" + name, list(shape), dt))

        sc = Sched(nc, es)
        op, dma = sc.op, sc.dma
        bar_n = [0]

        def barrier(old, new):
            col = 56 + (bar_n[0] % 8)
            bar_n[0] += 1
            op("dve", lambda e, col=col: e.memset(small[:, col:col + 1], 0.0), writes=list(old) + list(new))

        def pipeline(stages, n):
            ns = len(stages)
            for t in range(n + ns - 1):
                for k in range(ns - 1, -1, -1):
                    i = t - k
                    if 0 <= i < n:
                        stages[k](i)

        ident_f = sb("ident_f", [128, 128], F32)
        ident_b = sb("ident_b", [128, 128], BF16)
        fmask = sb("fmask", [128, 128], BF16)
        dbias = sb("dbias", [128, 4, 128], BF16)
        stri = sb("stri", [128, 128], BF16)
        ones_b = sb("ones_b", [128, 128], BF16)
        pcol = sb("pcol", [128, 1], F32)
        jgrid = sb("jgrid", [128, NB], F32)
        modr = sb("modr", [128, 6 * D], F32)
        A1 = modr[:, D:2 * D]
        A2 = modr[:, 4 * D:5 * D]
        subgs = sb("subgs", [128, 128], F32)
        nlam = sb("nlam", [128, 1], F32)
        BIGN = 92000
        big = sb("big", [128, BIGN], BF16)

        def carve(off, shape, dt):
            esz = 2 if dt == BF16 else 4
            n = 1
            for d_ in shape[1:]:
                n *= d_
            assert off % 4 == 0 and off + n * esz <= BIGN * 2, (off, shape)
            v = big[:, off // 2: off // 2 + n * esz // 2]
            if dt != BF16:
                v = v.bitcast(dt)
            if shape[0] != 128:
                v = v[0:shape[0]]
            if len(shape) == 3:
                v = v.rearrange("p (a b) -> p a b", a=shape[1])
            return v
        hT = carve(0, [128, 8, S], BF16)
        mixb = carve(65536, [128, NT * D], BF16)
        mix = mixb.rearrange("p (t d) -> p t d", t=NT)
        arena = carve(65536, [128, 16384], F32)
        small = sb("small", [128, 64], F32)
        L = carve(57344, [128, NT, 36], F32)

        pbig = ps("pbig", [128, 8 * 512], F32)
        pb = [pbig[:, i * 512:(i + 1) * 512] for i in range(8)]

        dma("sp", lambda e: e.dma_start(out=ident_f[:], in_=ident_d), writes=["ident_f"])
        dma("pool", lambda e: e.dma_start(out=ident_b[:], in_=ident_d), writes=["ident_b"])
        dma("pool", lambda e: e.dma_start(out=fmask[:], in_=fmask_d), writes=["fmask"])
        dma("pool", lambda e: e.dma_start(out=dbias[:], in_=dbias_d), writes=["dbias"])
        dma("pool", lambda e: e.dma_start(out=stri[:], in_=stri_d), writes=["stri"])
        dma("sp", lambda e: e.dma_start(out=pcol[:], in_=pcol_d), writes=["pcol"])
        dma("sp", lambda e: e.dma_start(out=jgrid[:], in_=jgrid_d), writes=["jgrid"])
        op("dve", lambda e: e.memset(ones_b[:], 1.0), writes=["ones_b"])

        try:
            crep_sb = arena[:, 0:1024].rearrange("p (c m) -> p c m", c=8)
            dma("sp", lambda e: e.dma_start(out=crep_sb, in_=crep), writes=["crep"])
            adaw_v = ada_w.rearrange("(c p) n -> p c n", p=128)
            for n in range(12):
                wt = arena[:, 1024 + (n % 2) * 4096: 1024 + (n % 2 + 1) * 4096].rearrange("p (c m) -> p c m", c=8)
                bt = arena[:, 9216 + (n % 2) * 512: 9216 + (n % 2 + 1) * 512]
                dma("sp", lambda e, wt=wt, n=n: e.dma_start(out=wt, in_=adaw_v[:, :, n * 512:(n + 1) * 512]),
                    writes=[("adaw", n % 2)])
                dma("sp", lambda e, bt=bt, n=n: e.dma_start(out=bt, in_=adab[:, n * 512:(n + 1) * 512]),
                    writes=[("adab", n % 2)])
                pbank = pb[n % 2]

                def mm(e, wt=wt, pbank=pbank):
                    for c in range(8):
                        ins = e.matmul(pbank[:], lhsT=crep_sb[:, c, :], rhs=wt[:, c, :], start=(c == 0), stop=(c == 7))
                    return ins
                op("pe", mm, reads=["crep", ("adaw", n % 2)], writes=[("pb", n % 2)])
                op("dve", lambda e, pbank=pbank, bt=bt, n=n: e.tensor_tensor(
                    out=modr[:, n * 512:(n + 1) * 512], in0=pbank[:], in1=bt, op=ALU.add),
                    reads=[("pb", n % 2), ("adab", n % 2)], writes=["modr"])
            g1t = arena[:, 10240:11264]
            g2t = arena[:, 11264:12288]
            dma("sp", lambda e: e.dma_start(out=g1t, in_=gvec[:, 0, :]), writes=["g1t"])
            dma("sp", lambda e: e.dma_start(out=g2t, in_=gvec[:, 1, :]), writes=["g2t"])
            op("dve", lambda e: e.scalar_tensor_tensor(out=A1, in0=modr[:, D:2 * D], scalar=1.0, in1=g1t,
                                                       op0=ALU.add, op1=ALU.mult), reads=["modr", "g1t"], writes=["A1"])
            op("dve", lambda e: e.scalar_tensor_tensor(out=A2, in0=modr[:, 4 * D:5 * D], scalar=1.0, in1=g2t,
                                                       op0=ALU.add, op1=ALU.mult), reads=["modr", "g2t"], writes=["A2"])
            SH1 = modr[:, 0:D]
            GATE1 = modr[:, 2 * D:3 * D]
            SH2 = modr[:, 3 * D:4 * D]
            GATE2 = modr[:, 5 * D:6 * D]
            lam_sb = arena[:, 12288:12544].rearrange("p (a b) -> p a b", a=4)
            sg_t = arena[:, 12544:12672]
            dma("sp", lambda e: e.dma_start(out=lam_sb, in_=lamv), writes=["lam_sb"])
            dma("sp", lambda e: e.dma_start(out=sg_t, in_=subg), writes=["sg_t"])
            junk64 = arena[:, 12672:12736]
            op("dve", lambda e: e.scalar_tensor_tensor(out=junk64, in0=lam_sb[:, 0, :], scalar=1.0, in1=lam_sb[:, 1, :],
                                                       op0=ALU.mult, op1=ALU.mult, accum_out=small[:, 0:1]),
               reads=["lam_sb"], writes=["junk64", ("small", 0)])
            op("dve", lambda e: e.scalar_tensor_tensor(out=junk64, in0=lam_sb[:, 2, :], scalar=1.0, in1=lam_sb[:, 3, :],
                                                       op0=ALU.mult, op1=ALU.mult, accum_out=small[:, 1:2]),
               reads=["lam_sb"], writes=["junk64", ("small", 1)])
            op("act", lambda e: e.activation(out=small[:, 2:4], in_=small[:, 0:2], func=AF.Exp),
               reads=[("small", 0), ("small", 1)], writes=[("small", 2)])
            op("dve", lambda e: e.tensor_tensor(out=small[:, 4:5], in0=small[:, 3:4], in1=small[:, 2:3], op=ALU.subtract),
               reads=[("small", 2)], writes=[("small", 4)])
            op("dve", lambda e: e.tensor_scalar(out=nlam[:], in0=small[:, 4:5], scalar1=-LAM_INIT, scalar2=None, op0=ALU.add),
               reads=[("small", 4)], writes=["nlam"])
            op("dve", lambda e: e.tensor_scalar(out=subgs[:], in0=sg_t, scalar1=1.0 - LAM_INIT, scalar2=None, op0=ALU.mult),
               reads=["sg_t"], writes=["subgs"])

            xv = x.rearrange("(t p) d -> t p d", p=128)
            xts = [carve(155648 + 4096 * k, [128, D], F32) for k in range(3)]
            hts = [carve(167936 + 4096 * k, [128, D], F32) for k in range(2)]
            hbs = [carve(176128 + 2048 * k, [128, D], BF16) for k in range(2)]
            jbB = carve(180224, [128, D], BF16)

            def b0(i):
                xt = xts[i % 3]
                dma("sp", lambda e, xt=xt, i=i: e.dma_start(out=xt, in_=xv[i]), writes=[("xt", i % 3)])

            def bA(i):
                xt = xts[i % 3]
                sq = small[:, 8 + (i % 2) * 4: 8 + (i % 2) * 4 + 4]
                kx, ks = ("xt", i % 3), ("sq", i % 2)
                op("act", lambda e, xt=xt, sq=sq: e.activation(out=jbB, in_=xt, func=AF.Square, accum_out=sq[:, 0:1]),
                   reads=[kx], writes=["jb", ks])
                op("act", lambda e, sq=sq: e.activation(out=sq[:, 1:2], in_=sq[:, 0:1], func=AF.Ln, scale=1.0 / D, bias=1e-6),
                   reads=[ks], writes=[ks])
                op("act", lambda e, sq=sq: e.activation(out=sq[:, 2:3], in_=sq[:, 1:2], func=AF.Exp, scale=-0.5),
                   reads=[ks], writes=[ks])

            def bB(i):
                xt, ht, hb = xts[i % 3], hts[i % 2], hbs[i % 2]
                sq = small[:, 8 + (i % 2) * 4: 8 + (i % 2) * 4 + 4]
                kx, kh, ks = ("xt", i % 3), ("hb", i % 2), ("sq", i % 2)
                op("dve", lambda e, xt=xt, ht=ht, sq=sq: e.scalar_tensor_tensor(out=ht, in0=xt, scalar=sq[:, 2:3], in1=A1,
                                                                                  op0=ALU.mult, op1=ALU.mult),
                   reads=[kx, ks, "A1"], writes=[("ht", i % 2)])
                op("dve", lambda e, ht=ht, hb=hb: e.tensor_tensor(out=hb, in0=ht, in1=SH1, op=ALU.add),
                   reads=[("ht", i % 2), "modr"], writes=[kh])
                tp = pb[2 + i % 2][:, :].bitcast(BF16)

                def tr(e, hb=hb, tp=tp):
                    for c in range(8):
                        ins = e.transpose(tp[:, c * 128:(c + 1) * 128], hb[:, c * 128:(c + 1) * 128], ident_b[:])
                    return ins
                op("pe", tr, reads=[kh, "ident_b"], writes=[("pb", 2 + i % 2)])

            def bC(i):
                tp = pb[2 + i % 2][:, :].bitcast(BF16)
                op("act", lambda e, tp=tp, i=i: e.copy(out=hT[:, :, i * 128:(i + 1) * 128],
                                                       in_=tp.rearrange("p (c t) -> p c t", c=8)),
                   reads=[("pb", 2 + i % 2)], writes=[("hT", i // 4)])
            pipeline([b0, bA, bB, bC], NT)

            wz = carve(182912, [128, 8, 8], BF16)
            nbf = carve(183040, [8, 1], F32)
            dma("pool", lambda e: e.dma_start(out=wz[:], in_=w_in.rearrange("(c p) n -> p c n", p=128)[:, :, 1536:1544]),
                writes=["wz"])
            dma("sp", lambda e: e.dma_start(out=nbf[:], in_=negbf), writes=["nbf"])
            op("dve", lambda e: e.tensor_scalar(out=nbf[:], in0=nbf[:], scalar1=-1.0, scalar2=None, op0=ALU.mult),
               reads=["nbf"], writes=["nbf"])
            E_t = arena[0:8, 0:4096]
            ones_t = arena[0:8, 4096:8192]
            cs_t = arena[0:8, 8192:12288]
            r_t = arena[0:8, 12288:16384]
            cpart = carve(131072, [8, 3, S], BF16)
            allB = ["crep", ("adaw", 0), ("adaw", 1), ("adab", 0), ("adab", 1), "g1t", "g2t", "lam_sb", "sg_t", "junk64",
                    ("xt", 0), ("xt", 1), ("xt", 2), ("hb", 0), ("hb", 1), "jb", ("ht", 0), ("ht", 1)]
            barrier(allB, ["ones_t", "E_t", "cs_t", "r_t"])
            op("dve", lambda e: e.memset(ones_t, 1.0), writes=["ones_t"])
            for n in range(8):
                pz = pb[n % 2]

                def mmz(e, pz=pz, n=n):
                    for c in range(8):
                        ins = e.matmul(pz[0:8, :], lhsT=wz[:, c, :], rhs=hT[:, c, n * 512:(n + 1) * 512],
                                       start=(c == 0), stop=(c == 7))
                    return ins
                op("pe", mmz, reads=["wz", ("hT", n)], writes=[("pb", n % 2)])
                op("act", lambda e, pz=pz, n=n: e.activation(out=E_t[:, n * 512:(n + 1) * 512], in_=pz[0:8, :], func=AF.Exp,
                                                             scale=-1.0, bias=nbf[:, 0:1]),
                   reads=[("pb", n % 2), "nbf"], writes=["E_t"])
            op("act", lambda e: e.activation(out=E_t, in_=E_t, func=AF.Ln, bias=1.0, scale=1.0), reads=["E_t"], writes=["E_t"])
            op("dve", lambda e: e.tensor_tensor_scan(out=cs_t, data0=ones_t, data1=E_t, initial=0.0, op0=ALU.mult, op1=ALU.add),
               reads=["E_t", "ones_t"], writes=["cs_t"])
            op("dve", lambda e: e.tensor_scalar(out=cpart[:, 0, :], in0=cs_t, scalar1=-1.0, scalar2=None, op0=ALU.mult),
               reads=["cs_t"], writes=["cpart"])
            op("dve", lambda e: e.scalar_tensor_tensor(out=r_t, in0=cs_t, scalar=-1.0, in1=cpart[:, 0, :], op0=ALU.mult,
                                                       op1=ALU.subtract), reads=["cs_t", "cpart"], writes=["r_t"])
            op("dve", lambda e: e.tensor_copy(out=cpart[:, 1, :], in_=r_t), reads=["r_t"], writes=["cpart"])
            op("dve", lambda e: e.tensor_tensor(out=r_t, in0=r_t, in1=cpart[:, 1, :], op=ALU.subtract),
               reads=["r_t", "cpart"], writes=["r_t"])
            op("dve", lambda e: e.tensor_copy(out=cpart[:, 2, :], in_=r_t), reads=["r_t"], writes=["cpart"])
            dma("sp", lambda e: e.dma_start(out=cumparts, in_=cpart[:]), reads=["cpart"], writes=["cumparts"])
            if DEBUG:
                dma("sp", lambda e: e.dma_start(out=dbg["cum"], in_=cs_t), reads=["cs_t"], is_out=True)
            barrier(["ones_t", "E_t", "cs_t", "r_t"], [("mix", i) for i in range(NT)])

            if STOP == 1:
                raise _Stop()
            sc.global_barrier()
            QA = carve(131072, [128, S], BF16)
            QB = carve(139264, [128, S], BF16)
            KA = carve(147456, [128, S], BF16)
            KB = carve(155648, [128, S], BF16)
            Vb = carve(163840, [128, NT * 130], BF16)
            wq = carve(172160, [128, 8, 128], BF16)
            wk = carve(174208, [128, 8, 128], BF16)
            wv = carve(176256, [128, 8, 128], BF16)
            PT = [carve(178304 + 2048 * i, [128, 1024], BF16) for i in range(2)]
            _a1b = modr[:, D:2 * D].bitcast(BF16)
            PT += [_a1b[:, 1024 * i:1024 * (i + 1)] for i in range(2)]
            O1n = modr[:, 0:512].rearrange("p (a b) -> p a b", a=4)
            dtmp = modr[:, 512:1024].rearrange("p (a b) -> p a b", a=4)
            junkd = carve(182400, [128, 128], F32)
            winv = w_in.rearrange("(c p) n -> p c n", p=128)
            op("dve", lambda e: e.memset(QB[0:64, :], 0.0), writes=["QBaug"])
            op("dve", lambda e: e.memset(KB[0:64, :], 0.0), writes=["KBaug"])
            op("dve", lambda e: e.memset(QA[64:128, :], 0.0), writes=["QAaug"])
            op("dve", lambda e: e.memset(KA[64:128, :], 0.0), writes=["KAaug"])

            NSB = 4
            LA = 3
            SB = [pb[0], pb[1], pb[2], pb[3]]
            OB = [[pb[4], pb[5]], [pb[6], pb[7]]]
            pjc = [0]

            def pjnext():
                k = pjc[0] % NSB
                pjc[0] += 1
                return pb[k], ("pb", k)

            def do_pair(pair):
                is_fox = pair < 4
                hd = pair - 4
                if is_fox:
                    qc, kc, vc = pair * 128, 512 + pair * 128, 1024 + pair * 128
                    DV = 64
                else:
                    qc, kc, vc = 1544 + hd * 128, 1544 + 512 + hd * 128, 1544 + 1024 + hd * 128
                    DV = 128
                W1 = DV + 1
                if is_fox:
                    NSBp, LAp = 6, 4
                    NSLOT_, LAU_ = 3, 2

                    def okeys_of(oset):
                        return [("pb", 6 + oset)]

                    def Oq_ap(oset, qb):
                        return pb[6 + oset][:, qb * W1:(qb + 1) * W1]
                else:
                    NSBp, LAp = 4, 2
                    NSLOT_, LAU_ = 4, 3

                    def okeys_of(oset):
                        return [("pb", 4 + 2 * oset), ("pb", 5 + 2 * oset)]

                    def Oq_ap(oset, qb):
                        return pb[4 + 2 * oset + qb // 2][:, (qb % 2) * W1:(qb % 2 + 1) * W1]
                dma("pool", lambda e, qc=qc: e.dma_start(out=wq[:], in_=winv[:, :, qc:qc + 128]), writes=["wq"])
                dma("pool", lambda e, kc=kc: e.dma_start(out=wk[:], in_=winv[:, :, kc:kc + 128]), writes=["wk"])
                dma("pool", lambda e, vc=vc: e.dma_start(out=wv[:], in_=winv[:, :, vc:vc + 128]), writes=["wv"])
                for ee in range(4 * pair, 4 * pair + 4):
                    dma("pool", lambda e, ee=ee: e.dma_start(
                        out=wgb[ee * 128:(ee + 1) * 128, :].rearrange("p (c f) -> p c f", c=8),
                        in_=w_gate[ee * 1024:(ee + 1) * 1024, :].rearrange("(c p) f -> p c f", p=128)),
                        writes=[("wconv", ee, 0)])
                    dma("pool", lambda e, ee=ee: e.dma_start(
                        out=wub[ee * 128:(ee + 1) * 128, :].rearrange("p (c f) -> p c f", c=8),
                        in_=w_up[ee * 1024:(ee + 1) * 1024, :].rearrange("(c p) f -> p c f", p=128)),
                        writes=[("wconv", ee, 1)])
                    dma("pool", lambda e, ee=ee: e.dma_start(
                        out=wdb[ee * 128:(ee + 1) * 128, :].rearrange("p (a f) -> p a f", a=4),
                        in_=w_down[ee * 512:(ee + 1) * 512, :].rearrange("(a p) f -> p a f", p=128)),
                        writes=[("wconv", ee, 2)])
                for n in range(8):
                    PJ, pjk = pjnext()

                    def mmq(e, n=n, PJ=PJ):
                        for c in range(8):
                            ins = e.matmul(PJ[:], lhsT=wq[:, c, :], rhs=hT[:, c, n * 512:(n + 1) * 512],
                                           start=(c == 0), stop=(c == 7))
                        return ins
                    op("pe", mmq, reads=["wq", ("hT", n)], writes=[pjk])
                    op("act", lambda e, n=n, PJ=PJ: e.activation(out=QA[0:64, n * 512:(n + 1) * 512], in_=PJ[0:64, :], func=AF.Copy,
                                                          scale=0.125), reads=[pjk], writes=["QA"])
                    op("dve", lambda e, n=n, PJ=PJ: e.tensor_scalar(out=QB[64:128, n * 512:(n + 1) * 512], in0=PJ[64:128, :],
                                                             scalar1=0.125, scalar2=None, op0=ALU.mult),
                       reads=[pjk], writes=["QB"])
                    PJ, pjk = pjnext()

                    def mmk(e, n=n, PJ=PJ):
                        for c in range(8):
                            ins = e.matmul(PJ[:], lhsT=wk[:, c, :], rhs=hT[:, c, n * 512:(n + 1) * 512],
                                           start=(c == 0), stop=(c == 7))
                        return ins
                    op("pe", mmk, reads=["wk", ("hT", n)], writes=[pjk])
                    op("act", lambda e, n=n, PJ=PJ: e.copy(out=KA[0:64, n * 512:(n + 1) * 512], in_=PJ[0:64, :]),
                       reads=[pjk], writes=["KA"])
                    op("dve", lambda e, n=n, PJ=PJ: e.tensor_copy(out=KB[64:128, n * 512:(n + 1) * 512], in_=PJ[64:128, :]),
                       reads=[pjk], writes=["KB"])
                if is_fox:
                    VA = Vb[:, 0:NT * 65].rearrange("p (t w) -> p t w", t=NT)
                    VBv = Vb[:, NT * 65:NT * 130].rearrange("p (t w) -> p t w", t=NT)
                    op("dve", lambda e, VA=VA: e.memset(VA[:, :, 64:65], 1.0), writes=["V"])
                    op("dve", lambda e, VBv=VBv: e.memset(VBv[:, :, 64:65], 1.0), writes=["V"])
                else:
                    VD = Vb[:, 0:NT * 129].rearrange("p (t w) -> p t w", t=NT)
                    op("dve", lambda e, VD=VD: e.memset(VD[:, :, 128:129], 1.0), writes=["V"])
                for g in range(8):
                    PJ, pjk = pjnext()

                    def mmv(e, g=g, PJ=PJ):
                        for t in range(4):
                            i = g * 4 + t
                            for c in range(8):
                                ins = e.matmul(PJ[:, t * 128:(t + 1) * 128], lhsT=hT[:, c, i * 128:(i + 1) * 128], rhs=wv[:, c, :],
                                               start=(c == 0), stop=(c == 7))
                        return ins
                    op("pe", mmv, reads=["wv", ("hT", g)], writes=[pjk])
                    pj3 = PJ[:, :].rearrange("p (t w) -> p t w", t=4)
                    if is_fox:
                        op("act", lambda e, g=g, VA=VA, pj3=pj3: e.copy(out=VA[:, g * 4:(g + 1) * 4, 0:64], in_=pj3[:, :, 0:64]),
                           reads=[pjk], writes=["V"])
                        op("dve", lambda e, g=g, VBv=VBv, pj3=pj3: e.tensor_copy(out=VBv[:, g * 4:(g + 1) * 4, 0:64],
                                                                                  in_=pj3[:, :, 64:128]),
                           reads=[pjk], writes=["V"])
                    else:
                        op("act", lambda e, g=g, VD=VD, pj3=pj3: e.copy(out=VD[:, g * 4:(g + 1) * 4, 0:128], in_=pj3),
                           reads=[pjk], writes=["V"])
                if is_fox:
                    ha, hb_ = 2 * pair, 2 * pair + 1
                    op("dve", lambda e: e.memset(QA[64:70, :], -1.0), writes=["QAaug"])
                    op("dve", lambda e: e.memset(KA[64:70, :], 1.0), writes=["KAaug"])
                    op("dve", lambda e: e.memset(QB[0:6, :], -1.0), writes=["QBaug"])
                    op("dve", lambda e: e.memset(KB[0:6, :], 1.0), writes=["KBaug"])
                    dma("sp", lambda e, ha=ha: e.dma_start(out=QA[64:67, :], in_=cumparts[ha]), reads=["cumparts"], writes=["QAaug"])
                    dma("sp", lambda e, ha=ha: e.dma_start(out=KA[67:70, :], in_=cumparts[ha]), reads=["cumparts"], writes=["KAaug"])
                    dma("sp", lambda e, hb_=hb_: e.dma_start(out=QB[0:3, :], in_=cumparts[hb_]), reads=["cumparts"], writes=["QBaug"])
                    dma("sp", lambda e, hb_=hb_: e.dma_start(out=KB[3:6, :], in_=cumparts[hb_]), reads=["cumparts"], writes=["KBaug"])
                else:
                    if hd == 0:
                        op("dve", lambda e: e.memset(QB[0:64, :], 0.0), writes=["QBaug"])
                        op("dve", lambda e: e.memset(KB[0:64, :], 0.0), writes=["KBaug"])
                        op("dve", lambda e: e.memset(QA[64:128, :], 0.0), writes=["QAaug"])
                        op("dve", lambda e: e.memset(KA[64:128, :], 0.0), writes=["KAaug"])
                    dma("pool", lambda e, hd=hd: e.dma_start(out=QA[64:68, :], in_=qaug_d[hd]), writes=["QAaug"])
                    dma("pool", lambda e, hd=hd: e.dma_start(out=KA[64:68, :], in_=kaug_d[hd]), writes=["KAaug"])
                    dma("pool", lambda e, hd=hd: e.dma_start(out=QB[0:4, :], in_=qaug_d[hd]), writes=["QBaug"])
                    dma("pool", lambda e, hd=hd: e.dma_start(out=KB[0:4, :], in_=kaug_d[hd]), writes=["KBaug"])

                maps = []
                for half in range(2):
                    if half == 0:
                        Qm, Km, lo, hi, dl, dh_ = QA, KA, 0, 128, 0, 64
                        rk = ["QA", "KA", "QAaug", "KAaug"]
                    else:
                        Qm, Km, lo, hi, dl, dh_ = QB, KB, 0, 128, 64, 128
                        rk = ["QB", "KB", "QBaug", "KBaug"]
                    if is_fox:
                        Vm = VA if half == 0 else VBv
                        dlo, dhi = lo, hi
                        dmask = fmask[:, :]
                    else:
                        Vm = VD
                        dlo, dhi = dl, dh_
                        dmask = dbias[:, hd, :]
                    maps.append((half, Qm, Km, lo, hi, dlo, dhi, dmask, Vm, rk))

                units = []
                for qt in range(8):
                    for m in maps:
                        if is_fox:
                            for j in range(0, 4 * qt, 2):
                                units.append((qt, m, [j, j + 1]))
                            for j in range(4 * qt, 4 * qt + 4):
                                units.append((qt, m, [j]))
                        else:
                            for j in range(4 * qt + 4):
                                units.append((qt, m, [j]))
                state = {"srot": 0, "prot": 0, "oset": 0}

                pending = []

                def zero_oset(okeys_):
                    for (_, bk) in okeys_:
                        op("dve", lambda e, bk=bk: e.memset(pb[bk][:, :], 0.0), writes=[("pb", bk)])

                def emit_qk(u, uidx):
                    qt, m, js = u
                    half, Qm, Km, lo, hi, dlo, dhi, dmask, Vm, rk = m
                    slot = uidx % NSLOT_
                    q0 = qt * 512
                    for k, j in enumerate(js):
                        bank = (2 * slot + k) if is_fox else slot
                        Sb = pb[bank]
                        jb = j - 4 * qt

                        def f(e, Sb=Sb, j=j, jb=jb):
                            if jb < 0:
                                return e.matmul(Sb[:, :], lhsT=Km[lo:hi, j * 128:(j + 1) * 128], rhs=Qm[lo:hi, q0:q0 + 512],
                                                start=True, stop=True)
                            c0 = jb * 128
                            e.matmul(Sb[:, c0:c0 + 128], lhsT=Km[dlo:dhi, j * 128:(j + 1) * 128],
                                     rhs=Qm[dlo:dhi, q0 + c0:q0 + c0 + 128], start=True, stop=False)
                            ins = e.matmul(Sb[:, c0:c0 + 128], lhsT=ident_b[:], rhs=dmask, start=False, stop=True)
                            if c0 + 128 < 512:
                                ins = e.matmul(Sb[:, c0 + 128:512], lhsT=Km[lo:hi, j * 128:(j + 1) * 128],
                                               rhs=Qm[lo:hi, q0 + c0 + 128:q0 + 512], start=True, stop=True, skip_group_check=True)
                            return ins
                        op("pe", f, reads=rk + ["ident_b", "fmask", "dbias"], writes=[("pb", bank)])

                def emit_rest(u, uidx):
                    qt, m, js = u
                    half, Qm, Km, lo, hi, dlo, dhi, dmask, Vm, rk = m
                    slot = uidx % NSLOT_
                    Pt = PT[uidx % 4]
                    oset = state["oset"]
                    okeys = okeys_of(oset)
                    state["ui"] = uidx
                    if len(js) == 2:
                        op("act", lambda e: e.activation(out=Pt[:, 0:1024], in_=pbig[:, 2 * slot * 512:2 * slot * 512 + 1024], func=AF.Exp),
                           reads=[("pb", 2 * slot), ("pb", 2 * slot + 1)], writes=[("PT", uidx % 4)])
                    else:
                        c0_ = max(js[0] - 4 * qt, 0) * 128
                        bank1 = (2 * slot) if is_fox else slot
                        op("act", lambda e: e.activation(out=Pt[:, c0_:512], in_=pb[bank1][:, c0_:512], func=AF.Exp),
                           reads=[("pb", bank1)], writes=[("PT", uidx % 4)])
                    for k, j in enumerate(js):
                        jb = j - 4 * qt

                        def pv(e, k=k, j=j, jb=jb):
                            if not is_fox:
                                e.matmul(pb[5 + 2 * oset][:, 384:512], lhsT=Pt[:, k * 512 + 384:k * 512 + 512],
                                         rhs=Vm[:, j, 0:128], start=False, stop=False, skip_group_check=True)
                            for qb in range(max(jb, 0), 4):
                                ins = e.matmul(Oq_ap(oset, qb), lhsT=Pt[:, k * 512 + qb * 128:k * 512 + (qb + 1) * 128],
                                               rhs=Vm[:, j, :], start=False, stop=(j == 4 * qt + qb), skip_group_check=True)
                            return ins
                        op("pe", pv, reads=[("PT", uidx % 4), "V"], writes=okeys)
                    if js[-1] == 4 * qt + 3:
                        finalize(qt, m, oset, okeys)
                        state["oset"] = 1 - oset

                def finalize(qt, m, oset, okeys):
                    half = m[0]
                    rec = small[:, 24:28]
                    for qb in range(4):
                        Oq = Oq_ap(oset, qb)
                        tile = 4 * qt + qb
                        op("dve", lambda e, Oq=Oq, qb=qb: e.reciprocal(out=rec[:, qb:qb + 1], in_=Oq[:, DV:DV + 1]),
                           reads=okeys, writes=[("rec", qb)])
                        if is_fox:
                            col = (2 * pair + half) * 64
                            op("dve", lambda e, Oq=Oq, qb=qb, tile=tile, col=col: e.tensor_scalar(
                                out=mix[:, tile, col:col + 64], in0=Oq[:, 0:64], scalar1=rec[:, qb:qb + 1], scalar2=None,
                                op0=ALU.mult), reads=okeys + [("rec", qb)], writes=[("mix", tile)])
                        elif half == 0:
                            op("dve", lambda e, Oq=Oq, qb=qb: e.tensor_scalar(
                                out=O1n[:, qb, :], in0=Oq[:, 0:128], scalar1=rec[:, qb:qb + 1], scalar2=None, op0=ALU.mult),
                                reads=okeys + [("rec", qb)], writes=[("O1n", qb)])
                        else:
                            op("dve", lambda e, qb=qb: e.tensor_tensor(out=rec[:, qb:qb + 1], in0=rec[:, qb:qb + 1], in1=nlam[:],
                                                                        op=ALU.mult), reads=[("rec", qb), "nlam"], writes=[("rec", qb)])
                            op("dve", lambda e, Oq=Oq, qb=qb: e.scalar_tensor_tensor(
                                out=dtmp[:, qb, :], in0=Oq[:, 0:128], scalar=rec[:, qb:qb + 1], in1=O1n[:, qb, :],
                                op0=ALU.mult, op1=ALU.add), reads=okeys + [("rec", qb), ("O1n", qb)], writes=[("dtmp", qb)])
                            op("dve", lambda e, qb=qb: e.scalar_tensor_tensor(
                                out=junkd[:], in0=dtmp[:, qb, :], scalar=1.0, in1=dtmp[:, qb, :], op0=ALU.mult,
                                op1=ALU.mult, accum_out=small[:, 28 + qb:29 + qb]), reads=[("dtmp", qb)], writes=["junkd", ("ssq", qb)])
                    zero_oset(okeys)
                    if (not is_fox) and half == 1:
                        def partB(qt=qt):
                            op("act", lambda e: e.activation(out=small[:, 32:36], in_=small[:, 28:32], func=AF.Ln, scale=1.0 / 128,
                                                             bias=1e-5), reads=[("ssq", q) for q in range(4)], writes=["lnss"])
                            op("act", lambda e: e.activation(out=small[:, 36:40], in_=small[:, 32:36], func=AF.Exp, scale=-0.5),
                               reads=["lnss"], writes=["rstd4"])
                            for qb in range(4):
                                tile = 4 * qt + qb
                                col = 512 + hd * 128
                                op("dve", lambda e, qb=qb, tile=tile, col=col: e.scalar_tensor_tensor(
                                    out=mix[:, tile, col:col + 128], in0=dtmp[:, qb, :], scalar=small[:, 36 + qb:37 + qb], in1=subgs[:],
                                    op0=ALU.mult, op1=ALU.mult), reads=[("dtmp", qb), "rstd4", "subgs"], writes=[("mix", tile)])
                        pending.append((state["ui"] + 4, partB))

                for os_ in range(2):
                    zero_oset(okeys_of(os_))
                for k0 in range(min(LAU_, len(units))):
                    emit_qk(units[k0], k0)
                for ui in range(len(units)):
                    if ui + LAU_ < len(units):
                        emit_qk(units[ui + LAU_], ui + LAU_)
                    emit_rest(units[ui], ui)
                    while pending and pending[0][0] <= ui:
                        pending.pop(0)[1]()
                while pending:
                    pending.pop(0)[1]()

            for pair_ in range(8):
                do_pair(pair_)

            if DEBUG:
                for i in range(NT):
                    tmpf = O1n[:, :, :].rearrange("p a b -> p (a b)")
                    for hh in range(2):
                        op("dve", lambda e, i=i, hh=hh: e.tensor_copy(out=tmpf, in_=mix[:, i, hh * 512:(hh + 1) * 512]),
                           reads=[("mix", i)], writes=["dbgt"])
                        dma("sp", lambda e, i=i, hh=hh: e.dma_start(out=dbg["mix"][i * 128:(i + 1) * 128, hh * 512:(hh + 1) * 512],
                                                                   in_=tmpf), reads=["dbgt"], is_out=True)

            if STOP == 2:
                raise _Stop()
            sc.global_barrier()
            wo = carve(0, [128, 8, D], BF16)
            wr = carve(16384, [128, 8, 36], F32)
            br = carve(17536, [128, 36], F32)
            mixT = [carve(17680 + 2048 * i, [128, 8, 128], BF16) for i in range(2)]
            xt2 = [carve(21776 + 4096 * i, [128, D], F32) for i in range(4)]
            h2T = [carve(38160 + 4096 * i, [128, 8, 128], F32) for i in range(2)]
            jb2 = carve(46352, [128, D], BF16)
            x1t = [carve(131072 + 4096 * i, [128, D], F32) for i in range(3)]
            h2f = [carve(143360 + 4096 * i, [128, D], F32) for i in range(3)]
            dma("pool", lambda e: e.dma_start(out=wo[:], in_=w_o.rearrange("(c p) n -> p c n", p=128)), writes=["wo"])
            dma("sp", lambda e: e.dma_start(out=wr[:], in_=w_r.rearrange("(c p) n -> p c n", p=128)), writes=["wr"])
            dma("sp", lambda e: e.dma_start(out=br[:], in_=b_r), writes=["br"])
            x1v = x1s.rearrange("(t p) d -> t p d", p=128)

            def d0(i):
                tpm = pb[0][:, :].bitcast(BF16)

                def trm(e, i=i, tpm=tpm):
                    for c in range(8):
                        ins = e.transpose(tpm[:, c * 128:(c + 1) * 128], mix[:, i, c * 128:(c + 1) * 128], ident_b[:])
                    return ins
                op("pe", trm, reads=[("mix", i), "ident_b"], writes=[("pb", 0)])
                dma("sp", lambda e, i=i: e.dma_start(out=xt2[i % 4][:], in_=xv[i]), writes=[("xt2", i % 4)])

            def d1(i):
                b2 = i % 2
                tpm = pb[0][:, :].bitcast(BF16)
                op("act", lambda e, tpm=tpm, b2=b2: e.copy(out=mixT[b2][:], in_=tpm.rearrange("p (c t) -> p c t", c=8)),
                   reads=[("pb", 0)], writes=[("mixT", b2)])

            def ybanks(i):
                return (1, 2) if i % 2 == 0 else (3, 4)

            def d2(i):
                b2 = i % 2
                for n in range(2):
                    bk = ybanks(i)[n]

                    def mmo(e, n=n, bk=bk, b2=b2):
                        for c in range(8):
                            ins = e.matmul(pb[bk][:], lhsT=mixT[b2][:, c, :], rhs=wo[:, c, n * 512:(n + 1) * 512],
                                           start=(c == 0), stop=(c == 7))
                        return ins
                    op("pe", mmo, reads=[("mixT", b2), "wo"], writes=[("pb", bk)])

            def d3(i):
                b3 = i % 3
                for n in range(2):
                    bk = ybanks(i)[n]
                    op("dve", lambda e, n=n, bk=bk, b3=b3: e.tensor_tensor(out=x1t[b3][:, n * 512:(n + 1) * 512], in0=pb[bk][:],
                                                                           in1=GATE1[:, n * 512:(n + 1) * 512], op=ALU.mult),
                       reads=[("pb", bk), "modr"], writes=[("x1t", b3)])
                op("dve", lambda e, i=i, b3=b3: e.tensor_tensor(out=x1t[b3][:], in0=x1t[b3][:], in1=xt2[i % 4][:], op=ALU.add),
                   reads=[("x1t", b3), ("xt2", i % 4)], writes=[("x1t", b3)])

            def d4(i):
                b2, b3 = i % 2, i % 3
                dma("sp", lambda e, i=i, b3=b3: e.dma_start(out=x1v[i], in_=x1t[b3][:]), reads=[("x1t", b3)], writes=[("x1s", i)])
                if DEBUG:
                    dma("sp", lambda e, i=i, b3=b3: e.dma_start(out=dbg["x1"][i * 128:(i + 1) * 128, :], in_=x1t[b3][:]),
                        reads=[("x1t", b3)], is_out=True)
                sq = small[:, 40 + b2 * 4: 44 + b2 * 4]
                ks = ("sq2", b2)
                op("act", lambda e, b3=b3, sq=sq: e.activation(out=jb2[:], in_=x1t[b3][:], func=AF.Square, accum_out=sq[:, 0:1]),
                   reads=[("x1t", b3)], writes=["jb2", ks])
                op("act", lambda e, sq=sq: e.activation(out=sq[:, 1:2], in_=sq[:, 0:1], func=AF.Ln, scale=1.0 / D, bias=1e-6),
                   reads=[ks], writes=[ks])
                op("act", lambda e, sq=sq: e.activation(out=sq[:, 2:3], in_=sq[:, 1:2], func=AF.Exp, scale=-0.5),
                   reads=[ks], writes=[ks])

            def d5(i):
                b2, b3 = i % 2, i % 3
                sq = small[:, 40 + b2 * 4: 44 + b2 * 4]
                ks = ("sq2", b2)
                op("dve", lambda e, b3=b3, sq=sq: e.scalar_tensor_tensor(out=h2f[b3][:], in0=x1t[b3][:], scalar=sq[:, 2:3],
                                                                          in1=A2, op0=ALU.mult, op1=ALU.mult),
                   reads=[("x1t", b3), ks, "A2"], writes=[("h2f", b3)])

            def d6(i):
                b3 = i % 3
                op("pool", lambda e, b3=b3: e.tensor_tensor(out=h2f[b3][:], in0=h2f[b3][:], in1=SH2, op=ALU.add),
                   reads=[("h2f", b3), "modr"], writes=[("h2f", b3)])

            def d7(i):
                b3 = i % 3
                op("act", lambda e, i=i, b3=b3: e.copy(out=mix[:, i, :], in_=h2f[b3][:]), reads=[("h2f", b3)], writes=[("mix", i)])
                for hh in range(2):
                    tb = pb[5 + hh]

                    def trh(e, hh=hh, tb=tb, b3=b3):
                        for c in range(4):
                            cc = hh * 4 + c
                            ins = e.transpose(tb[:, c * 128:(c + 1) * 128], h2f[b3][:, cc * 128:(cc + 1) * 128], ident_f[:])
                        return ins
                    op("pe", trh, reads=[("h2f", b3), "ident_f"], writes=[("pb", 5 + hh)])

            def d8(i):
                b2 = i % 2
                op("dve", lambda e, b2=b2: e.tensor_copy(out=h2T[b2][:, 0:4, :], in_=pb[5][:, :].rearrange("p (c t) -> p c t", c=4)),
                   reads=[("pb", 5)], writes=[("h2T", b2, 0)])
                op("act", lambda e, b2=b2: e.copy(out=h2T[b2][:, 4:8, :], in_=pb[6][:, :].rearrange("p (c t) -> p c t", c=4)),
                   reads=[("pb", 6)], writes=[("h2T", b2, 1)])

            def d9(i):
                b2 = i % 2

                def mml(e, b2=b2):
                    for c in range(8):
                        ins = e.matmul(pb[7][:, 0:36], lhsT=h2T[b2][:, c, :], rhs=wr[:, c, :], start=(c == 0), stop=(c == 7))
                    return ins
                op("pe", mml, reads=[("h2T", b2, 0), ("h2T", b2, 1), "wr"], writes=[("pb", 7)])

            def d10(i):
                op("dve", lambda e, i=i: e.tensor_tensor(out=L[:, i, :], in0=pb[7][:, 0:36], in1=br[:], op=ALU.add),
                   reads=[("pb", 7), "br"], writes=["L"])
            pipeline([d0, d1, d2, d3, d4, d5, d6, d7, d8, d9, d10], NT)

            if STOP == 3:
                raise _Stop()
            sc.global_barrier()
            R = {}
            roff = [131072]
            for nm, shp in [("gmax", [128, NT]), ("G", [128, NT, 4]), ("ge", [128, NT, 4]), ("gs", [128, NT]), ("pg", [128, NT]),
                            ("elm", [128, NT, 32]), ("v1", [128, NT]), ("M1", [128, NT, 32]), ("elm2", [128, NT, 32]),
                            ("v2", [128, NT]), ("M2", [128, NT, 32]), ("r", [128, NT]), ("w1", [128, NT]), ("w2", [128, NT]),
                            ("T", [128, NT, 32]), ("carry", [128, NT, 32]), ("pos", [128, NT, 32]), ("cnt", [128, 32]),
                            ("pad", [128, 32]), ("pends", [128, 32]), ("pst", [128, 32]), ("tmp3", [128, NT, 32]),
                            ("d1f", [128, NT]), ("d2f", [128, NT]), ("cmp", [128, NB, 32]), ("ebf", [128, NB]), ("tmpd", [128, NB]), ("skp", [128, NB]), ("ones32", [128, 32])]:
                if nm in ("w1", "w2"):
                    continue
                if nm == "cmp":
                    R[nm] = carve(0, shp, F32)
                    continue
                nbytes = 4
                for d_ in shp[1:]:
                    nbytes *= d_
                R[nm] = carve(roff[0], shp, F32)
                roff[0] += nbytes
            Mb = carve(roff[0], [128, NT, 32], BF16)
            roff[0] += 2048
            padi = carve(roff[0], [128, 32], I32)
            roff[0] += 128
            assert roff[0] <= 179200
            R["w1"] = carve(179200, [128, NT], F32)
            R["w2"] = carve(179328, [128, NT], F32)
            d1u = carve(179456, [128, NT], U32)
            d2u = carve(179584, [128, NT], U32)
            widx = carve(179712, [128, NB], U32)

            def bc_last(ap2, n):
                return ap2.unsqueeze(2).to_broadcast([128, ap2.shape[1], n])

            def dv(fn, reads, writes):
                return op("dve", fn, reads=reads, writes=writes)
            gl = L[:, :, 0:4]
            el = L[:, :, 4:36]
            dv(lambda e: e.tensor_reduce(out=R["gmax"][:], in_=gl, axis=AX.X, op=ALU.max), ["L"], ["gmax"])
            dv(lambda e: e.tensor_tensor(out=R["G"][:], in0=gl, in1=bc_last(R["gmax"][:], 4), op=ALU.is_equal), ["L", "gmax"], ["G"])
            dv(lambda e: e.tensor_tensor(out=R["ge"][:], in0=gl, in1=bc_last(R["gmax"][:], 4), op=ALU.subtract), ["L", "gmax"], ["ge"])
            op("act", lambda e: e.activation(out=R["ge"][:], in_=R["ge"][:], func=AF.Exp), reads=["ge"], writes=["ge"])
            dv(lambda e: e.tensor_reduce(out=R["gs"][:], in_=R["ge"][:], axis=AX.X, op=ALU.add), ["ge"], ["gs"])
            dv(lambda e: e.reciprocal(out=R["pg"][:], in_=R["gs"][:]), ["gs"], ["pg"])
            dv(lambda e: e.tensor_scalar(out=R["G"][:], in0=R["G"][:], scalar1=-1.0, scalar2=1e4, op0=ALU.add, op1=ALU.mult),
               ["G"], ["G"])
            for g in range(4):
                dv(lambda e, g=g: e.tensor_tensor(out=R["elm"][:, :, g * 8:(g + 1) * 8], in0=el[:, :, g * 8:(g + 1) * 8],
                                                  in1=bc_last(R["G"][:, :, g], 8), op=ALU.add), ["L", "G"], ["elm"])
            dv(lambda e: e.tensor_reduce(out=R["v1"][:], in_=R["elm"][:], axis=AX.X, op=ALU.max), ["elm"], ["v1"])
            dv(lambda e: e.tensor_tensor(out=R["M1"][:], in0=R["elm"][:], in1=bc_last(R["v1"][:], 32), op=ALU.is_equal),
               ["elm", "v1"], ["M1"])
            dv(lambda e: e.scalar_tensor_tensor(out=R["elm2"][:], in0=R["M1"][:], scalar=-1e4, in1=R["elm"][:], op0=ALU.mult,
                                                op1=ALU.add), ["M1", "elm"], ["elm2"])
            dv(lambda e: e.tensor_reduce(out=R["v2"][:], in_=R["elm2"][:], axis=AX.X, op=ALU.max), ["elm2"], ["v2"])
            dv(lambda e: e.tensor_tensor(out=R["M2"][:], in0=R["elm2"][:], in1=bc_last(R["v2"][:], 32), op=ALU.is_equal),
               ["elm2", "v2"], ["M2"])
            dv(lambda e: e.tensor_tensor(out=R["r"][:], in0=R["v2"][:], in1=R["v1"][:], op=ALU.subtract), ["v1", "v2"], ["r"])
            op("act", lambda e: e.activation(out=R["r"][:], in_=R["r"][:], func=AF.Exp), reads=["r"], writes=["r"])
            dv(lambda e: e.tensor_scalar(out=R["w1"][:], in0=R["r"][:], scalar1=1.0, scalar2=None, op0=ALU.add), ["r"], ["w1"])
            dv(lambda e: e.reciprocal(out=R["w1"][:], in_=R["w1"][:]), ["w1"], ["w1"])
            dv(lambda e: e.tensor_tensor(out=R["w1"][:], in0=R["w1"][:], in1=R["pg"][:], op=ALU.mult), ["w1", "pg"], ["w1"])
            dv(lambda e: e.tensor_tensor(out=R["w2"][:], in0=R["w1"][:], in1=R["r"][:], op=ALU.mult), ["w1", "r"], ["w2"])
            dv(lambda e: e.tensor_tensor(out=Mb[:], in0=R["M1"][:], in1=R["M2"][:], op=ALU.add), ["M1", "M2"], ["Mb"])

            def mmT(e):
                for i in range(NT):
                    ins = e.matmul(pb[i // 16][:, (i % 16) * 32:(i % 16 + 1) * 32], lhsT=ones_b[:], rhs=Mb[:, i, :],
                                   start=True, stop=True, skip_group_check=True)
                return ins
            op("pe", mmT, reads=["ones_b", "Mb"], writes=[("pb", 0), ("pb", 1)])
            for hh in range(2):
                dv(lambda e, hh=hh: e.tensor_copy(out=R["T"][:, hh * 16:(hh + 1) * 16, :],
                                                  in_=pb[hh][:, :].rearrange("p (t k) -> p t k", t=16)), [("pb", hh)], ["T"])

            def mmP(e):
                for i in range(NT):
                    ins = e.matmul(pb[2 + i // 16][:, (i % 16) * 32:(i % 16 + 1) * 32], lhsT=stri[:], rhs=Mb[:, i, :],
                                   start=True, stop=True, skip_group_check=True)
                return ins
            op("pe", mmP, reads=["stri", "Mb"], writes=[("pb", 2), ("pb", 3)])
            Tem, Cem, rmask = R["elm"], R["elm2"], R["tmp3"]
            flat = lambda ap3: ap3[:, :, :].rearrange("p a b -> p (a b)")
            dv(lambda e: e.tensor_copy(out=Tem[:, :, :], in_=R["T"][:, :, :].rearrange("p t k -> p k t")), ["T", "M1", "M2"], ["elm"])
            dv(lambda e: e.memset(rmask[:, :, :], 1.0), [], ["tmp3"])
            dv(lambda e: e.memset(rmask[:, :, 0:1], 0.0), ["tmp3"], ["tmp3"])
            dv(lambda e: e.tensor_tensor_scan(out=flat(Cem), data0=flat(rmask), data1=flat(Tem), initial=0.0,
                                              op0=ALU.mult, op1=ALU.add), ["elm", "tmp3", "M2", "v2"], ["elm2"])
            dv(lambda e: e.tensor_copy(out=R["cnt"][:], in_=Cem[:, :, NT - 1]), ["elm2"], ["cnt"])
            dv(lambda e: e.tensor_tensor(out=Tem[:, :, :], in0=Cem[:, :, :], in1=Tem[:, :, :], op=ALU.subtract), ["elm2", "elm"], ["elm"])
            dv(lambda e: e.tensor_copy(out=R["carry"][:, :, :], in_=Tem[:, :, :].rearrange("p k t -> p t k")), ["elm"], ["carry"])
            for hh in range(2):
                dv(lambda e, hh=hh: e.tensor_tensor(out=R["pos"][:, hh * 16:(hh + 1) * 16, :],
                                                    in0=pb[2 + hh][:, :].rearrange("p (t k) -> p t k", t=16),
                                                    in1=R["carry"][:, hh * 16:(hh + 1) * 16, :], op=ALU.add),
                   [("pb", 2 + hh), "carry"], ["pos"])
            dv(lambda e: e.tensor_scalar(out=padi[:], in0=R["cnt"][:], scalar1=float(BLK - 1), scalar2=None, op0=ALU.add),
               ["cnt"], ["padi"])
            dv(lambda e: e.tensor_scalar(out=padi[:], in0=padi[:], scalar1=7, scalar2=7, op0=ALU.arith_shift_right,
                                         op1=ALU.logical_shift_left), ["padi"], ["padi"])
            dv(lambda e: e.tensor_copy(out=R["pad"][:], in_=padi[:]), ["padi"], ["pad"])
            dv(lambda e: e.memset(R["ones32"][:], 1.0), [], ["ones32"])
            dv(lambda e: e.tensor_tensor_scan(out=R["pends"][:], data0=R["ones32"][:], data1=R["pad"][:], initial=0.0,
                                              op0=ALU.mult, op1=ALU.add), ["pad", "ones32"], ["pends"])
            dv(lambda e: e.tensor_tensor(out=R["pst"][:], in0=R["pends"][:], in1=R["pad"][:], op=ALU.subtract),
               ["pends", "pad"], ["pst"])
            dv(lambda e: e.tensor_tensor(out=R["pos"][:], in0=R["pos"][:], in1=R["pst"][:].unsqueeze(1).to_broadcast([128, NT, 32]),
                                         op=ALU.add), ["pos", "pst"], ["pos"])
            for Mk, dk, du in (("M1", "d1f", d1u), ("M2", "d2f", d2u)):
                dv(lambda e, Mk=Mk: e.tensor_tensor(out=R["tmp3"][:], in0=R[Mk][:], in1=R["pos"][:], op=ALU.mult),
                   [Mk, "pos"], ["tmp3"])
                dv(lambda e, dk=dk: e.tensor_reduce(out=R[dk][:], in_=R["tmp3"][:], axis=AX.X, op=ALU.add), ["tmp3"], [dk])
                dv(lambda e, dk=dk, du=du: e.tensor_copy(out=du[:], in_=R[dk][:]), [dk], [dk + "u"])
            dv(lambda e: e.tensor_tensor(out=R["cmp"][:], in0=R["pends"][:].unsqueeze(1).to_broadcast([128, NB, 32]),
                                         in1=bc_last(jgrid[:], 32), op=ALU.is_le), ["pends", "jgrid"], ["cmp"])
            dv(lambda e: e.tensor_reduce(out=R["ebf"][:], in_=R["cmp"][:], axis=AX.X, op=ALU.add), ["cmp"], ["ebf"])
            dv(lambda e: e.memset(R["skp"][:], 0.0), [], ["skp"])
            dv(lambda e: e.tensor_tensor(out=R["skp"][:, 1:NB], in0=R["ebf"][:, 1:NB], in1=R["ebf"][:, 0:NB - 1], op=ALU.is_equal),
               ["ebf", "skp"], ["skp"])
            for st_ in range(1, NSTR):
                c0_ = st_ * (NB // NSTR)
                dv(lambda e, c0_=c0_: e.memset(R["skp"][:, c0_:c0_ + 1], 0.0), ["skp"], ["skp"])
            dv(lambda e: e.tensor_scalar(out=R["skp"][:], in0=R["skp"][:], scalar1=1.0e6, scalar2=pcol[:, 0:1], op0=ALU.mult,
                                         op1=ALU.add), ["skp", "pcol"], ["skp"])
            dv(lambda e: e.scalar_tensor_tensor(out=R["tmpd"][:], in0=R["ebf"][:], scalar=128.0, in1=R["skp"][:], op0=ALU.mult,
                                                op1=ALU.add), ["ebf", "skp"], ["tmpd"])
            dv(lambda e: e.tensor_copy(out=widx[:], in_=R["tmpd"][:]), ["tmpd"], ["widx"])
            if DEBUG:
                rt = carve(180224, [128, NT, 4], F32)
                dv(lambda e: e.tensor_copy(out=rt[:, :, 0], in_=R["d1f"][:]), ["d1f"], ["rt"])
                dv(lambda e: e.tensor_copy(out=rt[:, :, 1], in_=R["d2f"][:]), ["d2f"], ["rt"])
                dv(lambda e: e.tensor_copy(out=rt[:, :, 2], in_=R["w1"][:]), ["w1"], ["rt"])
                dv(lambda e: e.tensor_copy(out=rt[:, :, 3], in_=R["w2"][:]), ["w2"], ["rt"])
                dma("sp", lambda e: e.dma_start(out=dbg["rt"], in_=rt[:]), reads=["rt"], is_out=True)

            if STOP == 4:
                raise _Stop()
            for i in range(NT):
                for du, dk in ((d1u, "d1fu"), (d2u, "d2fu")):
                    dma("pool", lambda e, i=i, du=du: e.indirect_dma_start(
                        out=xs[:, :], out_offset=bass.IndirectOffsetOnAxis(ap=du[:, i:i + 1], axis=0),
                        in_=mix[:, i, :], in_offset=None), reads=[("mix", i), dk], writes=[("xs", i, dk)])

            if STOP == 5:
                raise _Stop()
            sc.global_barrier()
            wg = [carve(0 + 8192 * i, [128, 8, 512], BF16) for i in range(NSTR)]
            wu = [carve(32768 + 8192 * i, [128, 8, 512], BF16) for i in range(NSTR)]
            wd = [carve(65536 + 8192 * i, [128, 4, D], BF16) for i in range(NSTR)]
            xb = [carve(98304 + 2048 * i, [128, D], BF16) for i in range(2)]
            xT = [carve(102400 + 2048 * i, [128, 8, BLK], BF16) for i in range(2)]
            hidT = [carve(106496 + 1024 * i, [128, 4, BLK], BF16) for i in range(2)]
            sg = [carve(108544 + 512 * i, [128, BLK], F32) for i in range(2)]
            yo = [carve(131072 + 2048 * i, [128, D], BF16) for i in range(2)]
            xsv = xs.rearrange("(j p) d -> j p d", p=128)
            ysv = ys.rearrange("(j p) d -> j p d", p=128)
            order = [k + (NB // NSTR) * st_ for k in range(NB // NSTR) for st_ in range(NSTR)]
            bregs = {}
            sc.streams["pool"].append(lambda e: bregs.update(g=e.to_reg(32 * 128 - 1)))

            def gA(pi):
                j, ws, b2 = order[pi], pi % NSTR, pi % 2
                for (wsb, wdr, nm) in ((wg[ws], wgb, "wg"), (wu[ws], wub, "wu"), (wd[ws], wdb, "wd")):
                    dma("pool", lambda e, wsb=wsb, wdr=wdr, j=j: e.indirect_dma_start(
                        out=wsb[:, :, :].rearrange("p a b -> p (a b)"), out_offset=None, in_=wdr[:, :],
                        in_offset=bass.IndirectOffsetOnAxis(ap=widx[:, j:j + 1], axis=0),
                        bounds_check=bregs["g"], oob_is_err=False),
                        reads=["widx"], writes=[(nm, ws)])
                dma("sp", lambda e, j=j, b2=b2: e.dma_start(out=xb[b2][:], in_=xsv[j]), reads=[], writes=[("xb", b2)])

            def gA1(pi):
                j, ws, b2 = order[pi], pi % NSTR, pi % 2
                for hh in range(2):
                    bank = 0 if hh == 0 else 7
                    tb = pb[bank][:, :].bitcast(BF16)[:, 0:512]

                    def trx(e, hh=hh, tb=tb, b2=b2):
                        for c in range(4):
                            cc = hh * 4 + c
                            ins = e.transpose(tb[:, c * 128:(c + 1) * 128], xb[b2][:, cc * 128:(cc + 1) * 128], ident_b[:])
                        return ins
                    op("pe", trx, reads=[("xb", b2), "ident_b"], writes=[("pb", bank)])
                    if hh == 0:
                        op("act", lambda e, hh=hh, tb=tb, b2=b2: e.copy(
                            out=xT[b2][:, hh * 4:(hh + 1) * 4, :], in_=tb.rearrange("p (c t) -> p c t", c=4)),
                           reads=[("pb", bank)], writes=[("xT", b2, hh)])
                    else:
                        op("dve", lambda e, hh=hh, tb=tb, b2=b2: e.tensor_copy(
                            out=xT[b2][:, hh * 4:(hh + 1) * 4, :], in_=tb.rearrange("p (c t) -> p c t", c=4)),
                           reads=[("pb", bank)], writes=[("xT", b2, hh)])

            def gB(pi):
                j, ws, b2 = order[pi], pi % NSTR, pi % 2
                xtk = [("xT", b2, 0), ("xT", b2, 1)]
                for fc in range(4):
                    pg_, pu_ = pb[1 + fc % 2][:, 0:BLK], pb[1 + fc % 2][:, BLK:2 * BLK]
                    kg = ("pb", 1 + fc % 2)

                    def mmg(e, fc=fc, pg_=pg_, ws=ws, b2=b2):
                        for c in range(8):
                            ins = e.matmul(pg_, lhsT=wg[ws][:, c, fc * 128:(fc + 1) * 128], rhs=xT[b2][:, c, :],
                                           start=(c == 0), stop=(c == 7))
                        return ins

                    def mmu(e, fc=fc, pu_=pu_, ws=ws, b2=b2):
                        for c in range(8):
                            ins = e.matmul(pu_, lhsT=wu[ws][:, c, fc * 128:(fc + 1) * 128], rhs=xT[b2][:, c, :],
                                           start=(c == 0), stop=(c == 7))
                        return ins
                    op("pe", mmg, reads=[("wg", ws)] + xtk, writes=[kg])
                    op("pe", mmu, reads=[("wu", ws)] + xtk, writes=[kg])
                    op("act", lambda e, fc=fc, pg_=pg_: e.activation(out=sg[fc % 2][:], in_=pg_, func=AF.Silu),
                       reads=[kg], writes=[("sg", fc % 2)])
                    op("dve", lambda e, fc=fc, pu_=pu_, b2=b2: e.tensor_tensor(out=hidT[b2][:, fc, :], in0=sg[fc % 2][:],
                                                                               in1=pu_, op=ALU.mult),
                       reads=[("sg", fc % 2), kg], writes=[("hidT", b2, fc)])

            def gC(pi):
                j, ws, b2 = order[pi], pi % NSTR, pi % 2
                for n in range(2):
                    ybk = 3 + 2 * b2 + n
                    yb = pb[ybk]

                    def mmd(e, n=n, yb=yb, ws=ws, b2=b2):
                        for fc in range(4):
                            ins = e.matmul(yb[:], lhsT=hidT[b2][:, fc, :], rhs=wd[ws][:, fc, n * 512:(n + 1) * 512],
                                           start=(fc == 0), stop=(fc == 3))
                        return ins
                    op("pe", mmd, reads=[("hidT", b2, f) for f in range(4)] + [("wd", ws)], writes=[("pb", ybk)])
                    if n == 0:
                        op("act", lambda e, n=n, yb=yb, b2=b2: e.copy(out=yo[b2][:, n * 512:(n + 1) * 512], in_=yb[:]),
                           reads=[("pb", ybk)], writes=[("yo", b2, n)])
                    else:
                        op("dve", lambda e, n=n, yb=yb, b2=b2: e.tensor_copy(out=yo[b2][:, n * 512:(n + 1) * 512], in_=yb[:]),
                           reads=[("pb", ybk)], writes=[("yo", b2, n)])
                dma("sp", lambda e, j=j, b2=b2: e.dma_start(out=ysv[j], in_=yo[b2][:]),
                    reads=[("yo", b2, 0), ("yo", b2, 1)], writes=[("ys", j)])
            pipeline([gA, gA1, gB, gC], NB)

            if STOP == 6:
                raise _Stop()
            y1 = [carve(139264 + 2048 * i, [128, D], BF16) for i in range(2)]
            y2 = [carve(143360 + 2048 * i, [128, D], BF16) for i in range(3)]
            mo = [carve(149504 + 4096 * i, [128, D], F32) for i in range(3)]
            x1t_h = [carve(161792 + 4096 * i, [128, D], F32) for i in range(3)]
            h2f_h = [carve(174080, [128, D], F32), carve(126976, [128, D], F32)]
            jb2_h = carve(120832, [128, D], BF16)
            gf = carve(122880, [128, D], F32)
            dma("sp", lambda e: e.dma_start(out=gf, in_=gvec[:, 2, :]), writes=["gf"])
            ov = out.rearrange("(t p) d -> t p d", p=128)

            def h0(i):
                dma("pool", lambda e, i=i: e.indirect_dma_start(
                    out=y1[i % 2][:, :], out_offset=None, in_=ys[:, :],
                    in_offset=bass.IndirectOffsetOnAxis(ap=d1u[:, i:i + 1], axis=0)), reads=[("ys", j_) for j_ in range(NB)] + ["d1fu"], writes=[("y1", i % 2)])
                dma("pool", lambda e, i=i: e.indirect_dma_start(
                    out=y2[i % 3][:, :], out_offset=None, in_=ys[:, :],
                    in_offset=bass.IndirectOffsetOnAxis(ap=d2u[:, i:i + 1], axis=0)), reads=[("ys", j_) for j_ in range(NB)] + ["d2fu"], writes=[("y2", i % 3)])

            def h1(i):
                op("act", lambda e, i=i: e.activation(out=mo[i % 3][:], in_=y1[i % 2][:], func=AF.Copy, scale=R["w1"][:, i:i + 1]),
                   reads=[("y1", i % 2), "w1"], writes=[("mo", i % 3)])

            def h2(i):
                op("dve", lambda e, i=i: e.scalar_tensor_tensor(out=mo[i % 3][:], in0=y2[i % 3][:], scalar=R["w2"][:, i:i + 1],
                                                               in1=mo[i % 3][:], op0=ALU.mult, op1=ALU.add),
                   reads=[("y2", i % 3), "w2", ("mo", i % 3)], writes=[("mo", i % 3)])

            def h3(i):
                op("pool", lambda e, i=i: e.tensor_tensor(out=mo[i % 3][:], in0=mo[i % 3][:], in1=GATE2, op=ALU.mult),
                   reads=[("mo", i % 3), "modr"], writes=[("mo", i % 3)])
                dma("sp", lambda e, i=i: e.dma_start(out=x1t_h[i % 3][:], in_=x1v[i]), reads=[("x1s", i)], writes=[("x1t_h", i % 3)])

            def h4(i):
                op("dve", lambda e, i=i: e.tensor_tensor(out=x1t_h[i % 3][:], in0=x1t_h[i % 3][:], in1=mo[i % 3][:], op=ALU.add),
                   reads=[("mo", i % 3), ("x1t_h", i % 3)], writes=[("x1t_h", i % 3)])

            def h5(i):
                b2 = i % 2
                sq = small[:, 48 + b2 * 4: 52 + b2 * 4]
                ks = ("sq3", b2)
                op("act", lambda e, i=i, sq=sq: e.activation(out=jb2_h[:], in_=x1t_h[i % 3][:], func=AF.Square, accum_out=sq[:, 0:1]),
                   reads=[("x1t_h", i % 3)], writes=["jb2_h", ks])
                op("act", lambda e, sq=sq: e.activation(out=sq[:, 1:2], in_=sq[:, 0:1], func=AF.Ln, scale=1.0 / D, bias=1e-6),
                   reads=[ks], writes=[ks])
                op("act", lambda e, sq=sq: e.activation(out=sq[:, 2:3], in_=sq[:, 1:2], func=AF.Exp, scale=-0.5),
                   reads=[ks], writes=[ks])

            def h6(i):
                b2 = i % 2
                sq = small[:, 48 + b2 * 4: 52 + b2 * 4]
                ks = ("sq3", b2)
                op("dve", lambda e, i=i, b2=b2, sq=sq: e.scalar_tensor_tensor(out=h2f_h[b2][:], in0=x1t_h[i % 3][:], scalar=sq[:, 2:3],
                                                                               in1=gf, op0=ALU.mult, op1=ALU.mult),
                   reads=[("x1t_h", i % 3), ks, "gf"], writes=[("h2f_h", b2)])
                dma("sp", lambda e, i=i, b2=b2: e.dma_start(out=ov[i], in_=h2f_h[b2][:]), reads=[("h2f_h", b2)], is_out=True)
            pipeline([h0, h1, h2, h3, h4, h5, h6], NT)
        except _Stop:
            pass

        block = es.enter_context(nc.Block())
        sc.finish(block)
    return nc


def _consts():
    k = np.arange(128)[:, None]
    q = np.arange(128)[None, :]
    ident = np.eye(128, dtype=np.float32)
    fmask = np.where(k <= q, 0.0, NEG).astype(np.float32)
    slopes = [2.0 ** (-8.0 * (i + 1) / 4) for i in range(4)]
    dbias = np.zeros((128, 4, 128), np.float32)
    ok = (q // 64) >= (k // 64)
    for h in range(4):
        dbias[:, h, :] = np.where(ok, -slopes[h] * np.abs(q - k), NEG)
    t = np.arange(S)
    thi = (t // 64) * 64.0
    tlo = (t % 64) * 1.0
    qaug = np.zeros((4, 4, S), np.float32)
    kaug = np.zeros((4, 4, S), np.float32)
    for h in range(4):
        qaug[h, 0] = -slopes[h] * thi
        qaug[h, 1] = -slopes[h] * tlo
        qaug[h, 2] = 1.0
        qaug[h, 3] = 1.0
        kaug[h, 0] = 1.0
        kaug[h, 1] = 1.0
        kaug[h, 2] = slopes[h] * thi
        kaug[h, 3] = slopes[h] * tlo
    stri = (k < q).astype(np.float32)
    pcol = np.arange(128, dtype=np.float32).reshape(128, 1)
    jgrid = np.tile((np.arange(NB, dtype=np.float32) * BLK)[None, :], (128, 1))
    return dict(ident=ident, fmask=fmask, dbias=dbias, qaug=qaug, kaug=kaug, stri=stri, pcol=pcol, jgrid=jgrid)


def _rep(v):
    return np.ascontiguousarray(np.broadcast_to(np.asarray(v, np.float32).reshape(1, -1), (128, v.size)))


def make_in_maps(x, c, ada_w, ada_b, norm1_g, w_in, b_f, lam_q1, lam_k1, lam_q2, lam_k2, subln_g, w_o, norm2_g,
                 w_rg, b_rg, w_re, b_re, w_gate, w_up, w_down, norm_f_g):
    f = np.float32
    consts = _consts()
    shared = dict(
        ada_w=np.ascontiguousarray(ada_w[0], f),
        adab=_rep(ada_b[0]),
        gvec=np.ascontiguousarray(np.stack([_rep(norm1_g[0]), _rep(norm2_g[0]), _rep(norm_f_g)], axis=1)),
        w_in=np.ascontiguousarray(w_in[0], f),
        lamv=np.ascontiguousarray(np.stack([_rep(lam_q1[0]), _rep(lam_k1[0]), _rep(lam_q2[0]), _rep(lam_k2[0])], axis=1)),
        subg=_rep(subln_g[0]),
        w_o=np.ascontiguousarray(w_o[0], f),
        w_r=np.ascontiguousarray(np.concatenate([w_rg[0], w_re[0]], axis=1), f),
        b_r=_rep(np.concatenate([b_rg[0], b_re[0]])),
        w_gate=np.ascontiguousarray(w_gate[0], f).reshape(32 * 1024, 512),
        w_up=np.ascontiguousarray(w_up[0], f).reshape(32 * 1024, 512),
        w_down=np.ascontiguousarray(w_down[0], f).reshape(32 * 512, 1024),
    )
    shared.update(consts)
    shared["negbf"] = np.ascontiguousarray(np.asarray(b_f[0], f).reshape(8, 1))
    maps = []
    for b in range(8):
        m = dict(shared)
        m["x"] = np.ascontiguousarray(x[b], f)
        cb = np.asarray(c[b], f).reshape(8, 128)
        m["crep"] = np.ascontiguousarray(np.broadcast_to(cb.T[:, :, None], (128, 8, 128)))
        maps.append(m)
    return maps


_NC_CACHE = {}


def kernel(**inputs):
    inputs = {k: np.asarray(v) for k, v in inputs.items()}
    if "nc" not in _NC_CACHE:
        _NC_CACHE["nc"] = build_nc()
    nc = _NC_CACHE["nc"]
    in_maps = make_in_maps(**inputs)
    res = run_bass_kernel_spmd(nc, in_maps, core_ids=list(range(8)))
    kernel.last = res
    return np.stack([np.asarray(r["out"], np.float32) for r in res.results], axis=0)
```

```python
import numpy as np
from contextlib import ExitStack
import concourse.bass as bass
import concourse.mybir as mybir
from concourse.bass_utils import run_bass_kernel_spmd

F32 = mybir.dt.float32
BF16 = mybir.dt.bfloat16
U32 = mybir.dt.uint32
I32 = mybir.dt.int32
AF = mybir.ActivationFunctionType
ALU = mybir.AluOpType
AX = mybir.AxisListType

S = 4096
D = 1024
NT = 32
NEG = -30000.0
BLK = 128
NB = 96
NSLOT = NB * BLK
LAM_INIT = 0.8 - 0.6
DEBUG = False
STOP = 0
NBRUN = NB
GBAR = False
NSTR = 4


class _Stop(Exception):
    pass


class Sched:
    def __init__(self, nc, es, nd=12):
        self.nc = nc
        self.streams = {k: [] for k in ("pe", "act", "dve", "pool", "sp")}
        self.csem = {k: es.enter_context(nc.semaphore("c_" + k)) for k in ("pe", "act", "dve", "pool")}
        self.ccnt = {k: 0 for k in self.csem}
        self.nd = {"sp": nd, "pool": 12}
        self.dsem = {q: [es.enter_context(nc.semaphore("d_%s%d" % (q, i))) for i in range(self.nd[q])] for q in ("sp", "pool")}
        self.dcnt = {q: [0] * self.nd[q] for q in ("sp", "pool")}
        self.drr = {"sp": 0, "pool": 0}
        self.waited = {k: {} for k in self.streams}
        self.lastw = {}
        self.readers = {}
        self.out_tokens = []

    def _deps(self, reads, writes):
        deps = []
        for r in reads:
            t = self.lastw.get(r)
            if t is not None:
                deps.append(t)
        for w in writes:
            t = self.lastw.get(w)
            if t is not None:
                deps.append(t)
            deps.extend(self.readers.get(w, {}).values())
        return deps

    def _emit_waits(self, eng, deps):
        for (key, sem, val, src) in deps:
            if src == eng and eng == "pe":
                continue
            if self.waited[eng].get(key, 0) >= val:
                continue
            self.waited[eng][key] = val
            self.streams[eng].append(lambda e, sem=sem, val=val: e.wait_ge(sem, val))

    def _record(self, tok, reads, writes):
        for w in writes:
            self.lastw[w] = tok
            self.readers[w] = {}
        for r in reads:
            d = self.readers.setdefault(r, {})
            old = d.get(tok[0])
            if old is None or old[2] < tok[2]:
                d[tok[0]] = tok

    def op(self, eng, fn, reads=(), writes=()):
        deps = self._deps(reads, writes)
        self._emit_waits(eng, deps)
        self.ccnt[eng] += 1
        val = self.ccnt[eng]
        sem = self.csem[eng]
        self.streams[eng].append(lambda e, fn=fn, sem=sem: fn(e).then_inc(sem, 1))
        tok = (eng, sem, val, eng)
        self._record(tok, reads, writes)
        return tok

    def dma(self, q, fn, reads=(), writes=(), is_out=False):
        deps = self._deps(reads, writes)
        i = self.drr[q]
        self.drr[q] = (i + 1) % self.nd[q]
        sem = self.dsem[q][i]
        prev = self.dcnt[q][i]
        key = ("d", q, i)
        if prev > 0:
            deps.append((key, sem, prev, "dma"))
        self._emit_waits(q, deps)
        self.dcnt[q][i] = prev + 16
        self.streams[q].append(lambda e, fn=fn, sem=sem: fn(e).then_inc(sem, 16))
        tok = (key, sem, prev + 16, "dma")
        self._record(tok, reads, writes)
        if is_out:
            self.out_tokens.append(tok)
        return tok

    def global_barrier(self):
        toks = []
        for k in self.csem:
            if self.ccnt[k] > 0:
                toks.append((k, self.csem[k], self.ccnt[k], "all"))
        for q in ("sp", "pool"):
            for i in range(self.nd[q]):
                if self.dcnt[q][i] > 0:
                    toks.append((("d", q, i), self.dsem[q][i], self.dcnt[q][i], "dma"))
        for eng in self.streams:
            self._emit_waits(eng, toks)
        self.lastw = {}
        self.readers = {}

    def finish(self, block):
        self._emit_waits("sp", list(self.out_tokens))
        st = self.streams

        @block.tensor
        def _(e):
            for f in st["pe"]:
                f(e)

        @block.scalar
        def _(e):
            for f in st["act"]:
                f(e)

        @block.vector
        def _(e):
            for f in st["dve"]:
                f(e)

        @block.gpsimd
        def _(e):
            for f in st["pool"]:
                f(e)

        @block.sync
        def _(e):
            for f in st["sp"]:
                f(e)


def build_nc():
    nc = bass.Bass("TRN2", target_bir_lowering=False)

    def din(name, shape, dt=F32):
        return nc.dram_tensor(name, list(shape), dt, kind="ExternalInput").ap()

    x = din("x", [S, D])
    crep = din("crep", [128, 8, 128])
    ada_w = din("ada_w", [D, 6 * D])
    adab = din("adab", [128, 6 * D])
    gvec = din("gvec", [128, 3, D])
    w_in = din("w_in", [D, 3080])
    negbf = din("negbf", [8, 1])
    lamv = din("lamv", [128, 4, 64])
    subg = din("subg", [128, 128])
    w_o = din("w_o", [D, D])
    w_r = din("w_r", [D, 36])
    b_r = din("b_r", [128, 36])
    w_gate = din("w_gate", [32 * 1024, 512])
    w_up = din("w_up", [32 * 1024, 512])
    w_down = din("w_down", [32 * 512, 1024])
    ident_d = din("ident", [128, 128])
    fmask_d = din("fmask", [128, 128])
    dbias_d = din("dbias", [128, 4, 128])
    qaug_d = din("qaug", [4, 4, S])
    kaug_d = din("kaug", [4, 4, S])
    stri_d = din("stri", [128, 128])
    pcol_d = din("pcol", [128, 1])
    jgrid_d = din("jgrid", [128, NB])
    out = nc.dram_tensor("out", [S, D], F32, kind="ExternalOutput").ap()
    dbg = {}
    if DEBUG:
        dbg["mix"] = nc.dram_tensor("dbg_mix", [S, D], F32, kind="ExternalOutput").ap()
        dbg["x1"] = nc.dram_tensor("dbg_x1", [S, D], F32, kind="ExternalOutput").ap()
        dbg["rt"] = nc.dram_tensor("dbg_rt", [128, NT, 4], F32, kind="ExternalOutput").ap()
        dbg["cum"] = nc.dram_tensor("dbg_cum", [8, S], F32, kind="ExternalOutput").ap()
    cumparts = nc.dram_tensor("cumparts", [8, 3, S], BF16, kind="Internal").ap()
    xs = nc.dram_tensor("xs", [NSLOT, D], BF16, kind="Internal").ap()
    ys = nc.dram_tensor("ys", [NSLOT, D], BF16, kind="Internal").ap()
    x1s = nc.dram_tensor("x1s", [S, D], F32, kind="Internal").ap()
    wgb = nc.dram_tensor("wgb", [32 * 128, 8 * 512], BF16, kind="Internal").ap()
    wub = nc.dram_tensor("wub", [32 * 128, 8 * 512], BF16, kind="Internal").ap()
    wdb = nc.dram_tensor("wdb", [32 * 128, 4 * 1024], BF16, kind="Internal").ap()

    with ExitStack() as es:
        def sb(name, shape, dt):
            return es.enter_context(nc.sbuf_tensor("s_" + name, list(shape), dt))

        def ps(name, shape, dt=F32):
            return es.enter_context(nc.psum_tensor("p_" + name, list(shape), dt))

        sc = Sched(nc, es)
        op, dma = sc.op, sc.dma
        bar_n = [0]

        def barrier(old, new):
            col = 56 + (bar_n[0] % 8)
            bar_n[0] += 1
            op("dve", lambda e, col=col: e.memset(small[:, col:col + 1], 0.0), writes=list(old) + list(new))

        def pipeline(stages, n):
            ns = len(stages)
            for t in range(n + ns - 1):
                for k in range(ns - 1, -1, -1):
                    i = t - k
                    if 0 <= i < n:
                        stages[k](i)

        ident_f = sb("ident_f", [128, 128], F32)
        ident_b = sb("ident_b", [128, 128], BF16)
        fmask = sb("fmask", [128, 128], BF16)
        dbias = sb("dbias", [128, 4, 128], BF16)
        stri = sb("stri", [128, 128], BF16)
        ones_b = sb("ones_b", [128, 128], BF16)
        pcol = sb("pcol", [128, 1], F32)
        jgrid = sb("jgrid", [128, NB], F32)
        modr = sb("modr", [128, 6 * D], F32)
        A1 = modr[:, D:2 * D]
        A2 = modr[:, 4 * D:5 * D]
        subgs = sb("subgs", [128, 128], F32)
        nlam = sb("nlam", [128, 1], F32)
        BIGN = 92000
        big = sb("big", [128, BIGN], BF16)

        def carve(off, shape, dt):
            esz = 2 if dt == BF16 else 4
            n = 1
            for d_ in shape[1:]:
                n *= d_
            assert off % 4 == 0 and off + n * esz <= BIGN * 2, (off, shape)
            v = big[:, off // 2: off // 2 + n * esz // 2]
            if dt != BF16:
                v = v.bitcast(dt)
            if shape[0] != 128:
                v = v[0:shape[0]]
            if len(shape) == 3:
                v = v.rearrange("p (a b) -> p a b", a=shape[1])
            return v
        hT = carve(0, [128, 8, S], BF16)
        mixb = carve(65536, [128, NT * D], BF16)
        mix = mixb.rearrange("p (t d) -> p t d", t=NT)
        arena = carve(65536, [128, 16384], F32)
        small = sb("small", [128, 64], F32)
        L = carve(57344, [128, NT, 36], F32)

        pbig = ps("pbig", [128, 8 * 512], F32)
        pb = [pbig[:, i * 512:(i + 1) * 512] for i in range(8)]

        dma("sp", lambda e: e.dma_start(out=ident_f[:], in_=ident_d), writes=["ident_f"])
        dma("pool", lambda e: e.dma_start(out=ident_b[:], in_=ident_d), writes=["ident_b"])
        dma("pool", lambda e: e.dma_start(out=fmask[:], in_=fmask_d), writes=["fmask"])
        dma("pool", lambda e: e.dma_start(out=dbias[:], in_=dbias_d), writes=["dbias"])
        dma("pool", lambda e: e.dma_start(out=stri[:], in_=stri_d), writes=["stri"])
        dma("sp", lambda e: e.dma_start(out=pcol[:], in_=pcol_d), writes=["pcol"])
        dma("sp", lambda e: e.dma_start(out=jgrid[:], in_=jgrid_d), writes=["jgrid"])
        op("dve", lambda e: e.memset(ones_b[:], 1.0), writes=["ones_b"])

        try:
            crep_sb = arena[:, 0:1024].rearrange("p (c m) -> p c m", c=8)
            dma("sp", lambda e: e.dma_start(out=crep_sb, in_=crep), writes=["crep"])
            adaw_v = ada_w.rearrange("(c p) n -> p c n", p=128)
            for n in range(12):
                wt = arena[:, 1024 + (n % 2) * 4096: 1024 + (n % 2 + 1) * 4096].rearrange("p (c m) -> p c m", c=8)
                bt = arena[:, 9216 + (n % 2) * 512: 9216 + (n % 2 + 1) * 512]
                dma("sp", lambda e, wt=wt, n=n: e.dma_start(out=wt, in_=adaw_v[:, :, n * 512:(n + 1) * 512]),
                    writes=[("adaw", n % 2)])
                dma("sp", lambda e, bt=bt, n=n: e.dma_start(out=bt, in_=adab[:, n * 512:(n + 1) * 512]),
                    writes=[("adab", n % 2)])
                pbank = pb[n % 2]

                def mm(e, wt=wt, pbank=pbank):
                    for c in range(8):
                        ins = e.matmul(pbank[:], lhsT=crep_sb[:, c, :], rhs=wt[:, c, :], start=(c == 0), stop=(c == 7))
                    return ins
                op("pe", mm, reads=["crep", ("adaw", n % 2)], writes=[("pb", n % 2)])
                op("dve", lambda e, pbank=pbank, bt=bt, n=n: e.tensor_tensor(
                    out=modr[:, n * 512:(n + 1) * 512], in0=pbank[:], in1=bt, op=ALU.add),
                    reads=[("pb", n % 2), ("adab", n % 2)], writes=["modr"])
            g1t = arena[:, 10240:11264]
            g2t = arena[:, 11264:12288]
            dma("sp", lambda e: e.dma_start(out=g1t, in_=gvec[:, 0, :]), writes=["g1t"])
            dma("sp", lambda e: e.dma_start(out=g2t, in_=gvec[:, 1, :]), writes=["g2t"])
            op("dve", lambda e: e.scalar_tensor_tensor(out=A1, in0=modr[:, D:2 * D], scalar=1.0, in1=g1t,
                                                       op0=ALU.add, op1=ALU.mult), reads=["modr", "g1t"], writes=["A1"])
            op("dve", lambda e: e.scalar_tensor_tensor(out=A2, in0=modr[:, 4 * D:5 * D], scalar=1.0, in1=g2t,
                                                       op0=ALU.add, op1=ALU.mult), reads=["modr", "g2t"], writes=["A2"])
            SH1 = modr[:, 0:D]
            GATE1 = modr[:, 2 * D:3 * D]
            SH2 = modr[:, 3 * D:4 * D]
            GATE2 = modr[:, 5 * D:6 * D]
            lam_sb = arena[:, 12288:12544].rearrange("p (a b) -> p a b", a=4)
            sg_t = arena[:, 12544:12672]
            dma("sp", lambda e: e.dma_start(out=lam_sb, in_=lamv), writes=["lam_sb"])
            dma("sp", lambda e: e.dma_start(out=sg_t, in_=subg), writes=["sg_t"])
            junk64 = arena[:, 12672:12736]
            op("dve", lambda e: e.scalar_tensor_tensor(out=junk64, in0=lam_sb[:, 0, :], scalar=1.0, in1=lam_sb[:, 1, :],
                                                       op0=ALU.mult, op1=ALU.mult, accum_out=small[:, 0:1]),
               reads=["lam_sb"], writes=["junk64", ("small", 0)])
            op("dve", lambda e: e.scalar_tensor_tensor(out=junk64, in0=lam_sb[:, 2, :], scalar=1.0, in1=lam_sb[:, 3, :],
                                                       op0=ALU.mult, op1=ALU.mult, accum_out=small[:, 1:2]),
               reads=["lam_sb"], writes=["junk64", ("small", 1)])
            op("act", lambda e: e.activation(out=small[:, 2:4], in_=small[:, 0:2], func=AF.Exp),
               reads=[("small", 0), ("small", 1)], writes=[("small", 2)])
            op("dve", lambda e: e.tensor_tensor(out=small[:, 4:5], in0=small[:, 3:4], in1=small[:, 2:3], op=ALU.subtract),
               reads=[("small", 2)], writes=[("small", 4)])
            op("dve", lambda e: e.tensor_scalar(out=nlam[:], in0=small[:, 4:5], scalar1=-LAM_INIT, scalar2=None, op0=ALU.add),
               reads=[("small", 4)], writes=["nlam"])
            op("dve", lambda e: e.tensor_scalar(out=subgs[:], in0=sg_t, scalar1=1.0 - LAM_INIT, scalar2=None, op0=ALU.mult),
               reads=["sg_t"], writes=["subgs"])

            xv = x.rearrange("(t p) d -> t p d", p=128)
            xts = [carve(155648 + 4096 * k, [128, D], F32) for k in range(3)]
            hts = [carve(167936 + 4096 * k, [128, D], F32) for k in range(2)]
            hbs = [carve(176128 + 2048 * k, [128, D], BF16) for k in range(2)]
            jbB = carve(180224, [128, D], BF16)

            def b0(i):
                xt = xts[i % 3]
                dma("sp", lambda e, xt=xt, i=i: e.dma_start(out=xt, in_=xv[i]), writes=[("xt", i % 3)])

            def bA(i):
                xt = xts[i % 3]
                sq = small[:, 8 + (i % 2) * 4: 8 + (i % 2) * 4 + 4]
                kx, ks = ("xt", i % 3), ("sq", i % 2)
                op("act", lambda e, xt=xt, sq=sq: e.activation(out=jbB, in_=xt, func=AF.Square, accum_out=sq[:, 0:1]),
                   reads=[kx], writes=["jb", ks])
                op("act", lambda e, sq=sq: e.activation(out=sq[:, 1:2], in_=sq[:, 0:1], func=AF.Ln, scale=1.0 / D, bias=1e-6),
                   reads=[ks], writes=[ks])
                op("act", lambda e, sq=sq: e.activation(out=sq[:, 2:3], in_=sq[:, 1:2], func=AF.Exp, scale=-0.5),
                   reads=[ks], writes=[ks])

            def bB(i):
                xt, ht, hb = xts[i % 3], hts[i % 2], hbs[i % 2]
                sq = small[:, 8 + (i % 2) * 4: 8 + (i % 2) * 4 + 4]
                kx, kh, ks = ("xt", i % 3), ("hb", i % 2), ("sq", i % 2)
                op("dve", lambda e, xt=xt, ht=ht, sq=sq: e.scalar_tensor_tensor(out=ht, in0=xt, scalar=sq[:, 2:3], in1=A1,
                                                                                  op0=ALU.mult, op1=ALU.mult),
                   reads=[kx, ks, "A1"], writes=[("ht", i % 2)])
                op("dve", lambda e, ht=ht, hb=hb: e.tensor_tensor(out=hb, in0=ht, in1=SH1, op=ALU.add),
                   reads=[("ht", i % 2), "modr"], writes=[kh])
                tp = pb[2 + i % 2][:, :].bitcast(BF16)

                def tr(e, hb=hb, tp=tp):
                    for c in range(8):
                        ins = e.transpose(tp[:, c * 128:(c + 1) * 128], hb[:, c * 128:(c + 1) * 128], ident_b[:])
                    return ins
                op("pe", tr, reads=[kh, "ident_b"], writes=[("pb", 2 + i % 2)])

            def bC(i):
                tp = pb[2 + i % 2][:, :].bitcast(BF16)
                op("act", lambda e, tp=tp, i=i: e.copy(out=hT[:, :, i * 128:(i + 1) * 128],
                                                       in_=tp.rearrange("p (c t) -> p c t", c=8)),
                   reads=[("pb", 2 + i % 2)], writes=[("hT", i // 4)])
            pipeline([b0, bA, bB, bC], NT)

            wz = carve(182912, [128, 8, 8], BF16)
            nbf = carve(183040, [8, 1], F32)
            dma("pool", lambda e: e.dma_start(out=wz[:], in_=w_in.rearrange("(c p) n -> p c n", p=128)[:, :, 1536:1544]),
                writes=["wz"])
            dma("sp", lambda e: e.dma_start(out=nbf[:], in_=negbf), writes=["nbf"])
            op("dve", lambda e: e.tensor_scalar(out=nbf[:], in0=nbf[:], scalar1=-1.0, scalar2=None, op0=ALU.mult),
               reads=["nbf"], writes=["nbf"])
            E_t = arena[0:8, 0:4096]
            ones_t = arena[0:8, 4096:8192]
            cs_t = arena[0:8, 8192:12288]
            r_t = arena[0:8, 12288:16384]
            cpart = carve(131072, [8, 3, S], BF16)
            allB = ["crep", ("adaw", 0), ("adaw", 1), ("adab", 0), ("adab", 1), "g1t", "g2t", "lam_sb", "sg_t", "junk64",
                    ("xt", 0), ("xt", 1), ("xt", 2), ("hb", 0), ("hb", 1), "jb", ("ht", 0), ("ht", 1)]
            barrier(allB, ["ones_t", "E_t", "cs_t", "r_t"])
            op("dve", lambda e: e.memset(ones_t, 1.0), writes=["ones_t"])
            for n in range(8):
                pz = pb[n % 2]

                def mmz(e, pz=pz, n=n):
                    for c in range(8):
                        ins = e.matmul(pz[0:8, :], lhsT=wz[:, c, :], rhs=hT[:, c, n * 512:(n + 1) * 512],
                                       start=(c == 0), stop=(c == 7))
                    return ins
                op("pe", mmz, reads=["wz", ("hT", n)], writes=[("pb", n % 2)])
                op("act", lambda e, pz=pz, n=n: e.activation(out=E_t[:, n * 512:(n + 1) * 512], in_=pz[0:8, :], func=AF.Exp,
                                                             scale=-1.0, bias=nbf[:, 0:1]),
                   reads=[("pb", n % 2), "nbf"], writes=["E_t"])
            op("act", lambda e: e.activation(out=E_t, in_=E_t, func=AF.Ln, bias=1.0, scale=1.0), reads=["E_t"], writes=["E_t"])
            op("dve", lambda e: e.tensor_tensor_scan(out=cs_t, data0=ones_t, data1=E_t, initial=0.0, op0=ALU.mult, op1=ALU.add),
               reads=["E_t", "ones_t"], writes=["cs_t"])
            op("dve", lambda e: e.tensor_scalar(out=cpart[:, 0, :], in0=cs_t, scalar1=-1.0, scalar2=None, op0=ALU.mult),
               reads=["cs_t"], writes=["cpart"])
            op("dve", lambda e: e.scalar_tensor_tensor(out=r_t, in0=cs_t, scalar=-1.0, in1=cpart[:, 0, :], op0=ALU.mult,
                                                       op1=ALU.subtract), reads=["cs_t", "cpart"], writes=["r_t"])
            op("dve", lambda e: e.tensor_copy(out=cpart[:, 1, :], in_=r_t), reads=["r_t"], writes=["cpart"])
            op("dve", lambda e: e.tensor_tensor(out=r_t, in0=r_t, in1=cpart[:, 1, :], op=ALU.subtract),
               reads=["r_t", "cpart"], writes=["r_t"])
            op("dve", lambda e: e.tensor_copy(out=cpart[:, 2, :], in_=r_t), reads=["r_t"], writes=["cpart"])
            dma("sp", lambda e: e.dma_start(out=cumparts, in_=cpart[:]), reads=["cpart"], writes=["cumparts"])
            if DEBUG:
                dma("sp", lambda e: e.dma_start(out=dbg["cum"], in_=cs_t), reads=["cs_t"], is_out=True)
            barrier(["ones_t", "E_t", "cs_t", "r_t"], [("mix", i) for i in range(NT)])

            if STOP == 1:
                raise _Stop()
            sc.global_barrier()
            QA = carve(131072, [128, S], BF16)
            QB = carve(139264, [128, S], BF16)
            KA = carve(147456, [128, S], BF16)
            KB = carve(155648, [128, S], BF16)
            Vb = carve(163840, [128, NT * 130], BF16)
            wq = carve(172160, [128, 8, 128], BF16)
            wk = carve(174208, [128, 8, 128], BF16)
            wv = carve(176256, [128, 8, 128], BF16)
            PT = [carve(178304 + 2048 * i, [128, 1024], BF16) for i in range(2)]
            _a1b = modr[:, D:2 * D].bitcast(BF16)
            PT += [_a1b[:, 1024 * i:1024 * (i + 1)] for i in range(2)]
            O1n = modr[:, 0:512].rearrange("p (a b) -> p a b", a=4)
            dtmp = modr[:, 512:1024].rearrange("p (a b) -> p a b", a=4)
            junkd = carve(182400, [128, 128], F32)
            winv = w_in.rearrange("(c p) n -> p c n", p=128)
            op("dve", lambda e: e.memset(QB[0:64, :], 0.0), writes=["QBaug"])
            op("dve", lambda e: e.memset(KB[0:64, :], 0.0), writes=["KBaug"])
            op("dve", lambda e: e.memset(QA[64:128, :], 0.0), writes=["QAaug"])
            op("dve", lambda e: e.memset(KA[64:128, :], 0.0), writes=["KAaug"])

            NSB = 4
            LA = 3
            SB = [pb[0], pb[1], pb[2], pb[3]]
            OB = [[pb[4], pb[5]], [pb[6], pb[7]]]
            pjc = [0]

            def pjnext():
                k = pjc[0] % NSB
                pjc[0] += 1
                return pb[k], ("pb", k)

            def do_pair(pair):
                is_fox = pair < 4
                hd = pair - 4
                if is_fox:
                    qc, kc, vc = pair * 128, 512 + pair * 128, 1024 + pair * 128
                    DV = 64
                else:
                    qc, kc, vc = 1544 + hd * 128, 1544 + 512 + hd * 128, 1544 + 1024 + hd * 128
                    DV = 128
                W1 = DV + 1
                if is_fox:
                    NSBp, LAp = 6, 4
                    NSLOT_, LAU_ = 3, 2

                    def okeys_of(oset):
                        return [("pb", 6 + oset)]

                    def Oq_ap(oset, qb):
                        return pb[6 + oset][:, qb * W1:(qb + 1) * W1]
                else:
                    NSBp, LAp = 4, 2
                    NSLOT_, LAU_ = 4, 3

                    def okeys_of(oset):
                        return [("pb", 4 + 2 * oset), ("pb", 5 + 2 * oset)]

                    def Oq_ap(oset, qb):
                        return pb[4 + 2 * oset + qb // 2][:, (qb % 2) * W1:(qb % 2 + 1) * W1]
                dma("pool", lambda e, qc=qc: e.dma_start(out=wq[:], in_=winv[:, :, qc:qc + 128]), writes=["wq"])
                dma("pool", lambda e, kc=kc: e.dma_start(out=wk[:], in_=winv[:, :, kc:kc + 128]), writes=["wk"])
                dma("pool", lambda e, vc=vc: e.dma_start(out=wv[:], in_=winv[:, :, vc:vc + 128]), writes=["wv"])
                for ee in range(4 * pair, 4 * pair + 4):
                    dma("pool", lambda e, ee=ee: e.dma_start(
                        out=wgb[ee * 128:(ee + 1) * 128, :].rearrange("p (c f) -> p c f", c=8),
                        in_=w_gate[ee * 1024:(ee + 1) * 1024, :].rearrange("(c p) f -> p c f", p=128)),
                        writes=[("wconv", ee, 0)])
                    dma("pool", lambda e, ee=ee: e.dma_start(
                        out=wub[ee * 128:(ee + 1) * 128, :].rearrange("p (c f) -> p c f", c=8),
                        in_=w_up[ee * 1024:(ee + 1) * 1024, :].rearrange("(c p) f -> p c f", p=128)),
                        writes=[("wconv", ee, 1)])
                    dma("pool", lambda e, ee=ee: e.dma_start(
                        out=wdb[ee * 128:(ee + 1) * 128, :].rearrange("p (a f) -> p a f", a=4),
                        in_=w_down[ee * 512:(ee + 1) * 512, :].rearrange("(a p) f -> p a f", p=128)),
                        writes=[("wconv", ee, 2)])
                for n in range(8):
                    PJ, pjk = pjnext()

                    def mmq(e, n=n, PJ=PJ):
                        for c in range(8):
                            ins = e.matmul(PJ[:], lhsT=wq[:, c, :], rhs=hT[:, c, n * 512:(n + 1) * 512],
                                           start=(c == 0), stop=(c == 7))
                        return ins
                    op("pe", mmq, reads=["wq", ("hT", n)], writes=[pjk])
                    op("act", lambda e, n=n, PJ=PJ: e.activation(out=QA[0:64, n * 512:(n + 1) * 512], in_=PJ[0:64, :], func=AF.Copy,
                                                          scale=0.125), reads=[pjk], writes=["QA"])
                    op("dve", lambda e, n=n, PJ=PJ: e.tensor_scalar(out=QB[64:128, n * 512:(n + 1) * 512], in0=PJ[64:128, :],
                                                             scalar1=0.125, scalar2=None, op0=ALU.mult),
                       reads=[pjk], writes=["QB"])
                    PJ, pjk = pjnext()

                    def mmk(e, n=n, PJ=PJ):
                        for c in range(8):
                            ins = e.matmul(PJ[:], lhsT=wk[:, c, :], rhs=hT[:, c, n * 512:(n + 1) * 512],
                                           start=(c == 0), stop=(c == 7))
                        return ins
                    op("pe", mmk, reads=["wk", ("hT", n)], writes=[pjk])
                    op("act", lambda e, n=n, PJ=PJ: e.copy(out=KA[0:64, n * 512:(n + 1) * 512], in_=PJ[0:64, :]),
                       reads=[pjk], writes=["KA"])
                    op("dve", lambda e, n=n, PJ=PJ: e.tensor_copy(out=KB[64:128, n * 512:(n + 1) * 512], in_=PJ[64:128, :]),
                       reads=[pjk], writes=["KB"])
                if is_fox:
                    VA = Vb[:, 0:NT * 65].rearrange("p (t w) -> p t w", t=NT)
                    VBv = Vb[:, NT * 65:NT * 130].rearrange("p (t w) -> p t w", t=NT)
                    op("dve", lambda e, VA=VA: e.memset(VA[:, :, 64:65], 1.0), writes=["V"])
                    op("dve", lambda e, VBv=VBv: e.memset(VBv[:, :, 64:65], 1.0), writes=["V"])
                else:
                    VD = Vb[:, 0:NT * 129].rearrange("p (t w) -> p t w", t=NT)
                    op("dve", lambda e, VD=VD: e.memset(VD[:, :, 128:129], 1.0), writes=["V"])
                for g in range(8):
                    PJ, pjk = pjnext()

                    def mmv(e, g=g, PJ=PJ):
                        for t in range(4):
                            i = g * 4 + t
                            for c in range(8):
                                ins = e.matmul(PJ[:, t * 128:(t + 1) * 128], lhsT=hT[:, c, i * 128:(i + 1) * 128], rhs=wv[:, c, :],
                                               start=(c == 0), stop=(c == 7))
                        return ins
                    op("pe", mmv, reads=["wv", ("hT", g)], writes=[pjk])
                    pj3 = PJ[:, :].rearrange("p (t w) -> p t w", t=4)
                    if is_fox:
                        op("act", lambda e, g=g, VA=VA, pj3=pj3: e.copy(out=VA[:, g * 4:(g + 1) * 4, 0:64], in_=pj3[:, :, 0:64]),
                           reads=[pjk], writes=["V"])
                        op("dve", lambda e, g=g, VBv=VBv, pj3=pj3: e.tensor_copy(out=VBv[:, g * 4:(g + 1) * 4, 0:64],
                                                                                  in_=pj3[:, :, 64:128]),
                           reads=[pjk], writes=["V"])
                    else:
                        op("act", lambda e, g=g, VD=VD, pj3=pj3: e.copy(out=VD[:, g * 4:(g + 1) * 4, 0:128], in_=pj3),
                           reads=[pjk], writes=["V"])
                if is_fox:
                    ha, hb_ = 2 * pair, 2 * pair + 1
                    op("dve", lambda e: e.memset(QA[64:70, :], -1.0), writes=["QAaug"])
                    op("dve", lambda e: e.memset(KA[64:70, :], 1.0), writes=["KAaug"])
                    op("dve", lambda e: e.memset(QB[0:6, :], -1.0), writes=["QBaug"])
                    op("dve", lambda e: e.memset(KB[0:6, :], 1.0), writes=["KBaug"])
                    dma("sp", lambda e, ha=ha: e.dma_start(out=QA[64:67, :], in_=cumparts[ha]), reads=["cumparts"], writes=["QAaug"])
                    dma("sp", lambda e, ha=ha: e.dma_start(out=KA[67:70, :], in_=cumparts[ha]), reads=["cumparts"], writes=["KAaug"])
                    dma("sp", lambda e, hb_=hb_: e.dma_start(out=QB[0:3, :], in_=cumparts[hb_]), reads=["cumparts"], writes=["QBaug"])
                    dma("sp", lambda e, hb_=hb_: e.dma_start(out=KB[3:6, :], in_=cumparts[hb_]), reads=["cumparts"], writes=["KBaug"])
                else:
                    if hd == 0:
                        op("dve", lambda e: e.memset(QB[0:64, :], 0.0), writes=["QBaug"])
                        op("dve", lambda e: e.memset(KB[0:64, :], 0.0), writes=["KBaug"])
                        op("dve", lambda e: e.memset(QA[64:128, :], 0.0), writes=["QAaug"])
                        op("dve", lambda e: e.memset(KA[64:128, :], 0.0), writes=["KAaug"])
                    dma("pool", lambda e, hd=hd: e.dma_start(out=QA[64:68, :], in_=qaug_d[hd]), writes=["QAaug"])
                    dma("pool", lambda e, hd=hd: e.dma_start(out=KA[64:68, :], in_=kaug_d[hd]), writes=["KAaug"])
                    dma("pool", lambda e, hd=hd: e.dma_start(out=QB[0:4, :], in_=qaug_d[hd]), writes=["QBaug"])
                    dma("pool", lambda e, hd=hd: e.dma_start(out=KB[0:4, :], in_=kaug_d[hd]), writes=["KBaug"])

                maps = []
                for half in range(2):
                    if half == 0:
                        Qm, Km, lo, hi, dl, dh_ = QA, KA, 0, 128, 0, 64
                        rk = ["QA", "KA", "QAaug", "KAaug"]
                    else:
                        Qm, Km, lo, hi, dl, dh_ = QB, KB, 0, 128, 64, 128
                        rk = ["QB", "KB", "QBaug", "KBaug"]
                    if is_fox:
                        Vm = VA if half == 0 else VBv
                        dlo, dhi = lo, hi
                        dmask = fmask[:, :]
                    else:
                        Vm = VD
                        dlo, dhi = dl, dh_
                        dmask = dbias[:, hd, :]
                    maps.append((half, Qm, Km, lo, hi, dlo, dhi, dmask, Vm, rk))

                units = []
                for qt in range(8):
                    for m in maps:
                        if is_fox:
                            for j in range(0, 4 * qt, 2):
                                units.append((qt, m, [j, j + 1]))
                            for j in range(4 * qt, 4 * qt + 4):
                                units.append((qt, m, [j]))
                        else:
                            for j in range(4 * qt + 4):
                                units.append((qt, m, [j]))
                state = {"srot": 0, "prot": 0, "oset": 0}

                pending = []

                def zero_oset(okeys_):
                    for (_, bk) in okeys_:
                        op("dve", lambda e, bk=bk: e.memset(pb[bk][:, :], 0.0), writes=[("pb", bk)])

                def emit_qk(u, uidx):
                    qt, m, js = u
                    half, Qm, Km, lo, hi, dlo, dhi, dmask, Vm, rk = m
                    slot = uidx % NSLOT_
                    q0 = qt * 512
                    for k, j in enumerate(js):
                        bank = (2 * slot + k) if is_fox else slot
                        Sb = pb[bank]
                        jb = j - 4 * qt

                        def f(e, Sb=Sb, j=j, jb=jb):
                            if jb < 0:
                                return e.matmul(Sb[:, :], lhsT=Km[lo:hi, j * 128:(j + 1) * 128], rhs=Qm[lo:hi, q0:q0 + 512],
                                                start=True, stop=True)
                            c0 = jb * 128
                            e.matmul(Sb[:, c0:c0 + 128], lhsT=Km[dlo:dhi, j * 128:(j + 1) * 128],
                                     rhs=Qm[dlo:dhi, q0 + c0:q0 + c0 + 128], start=True, stop=False)
                            ins = e.matmul(Sb[:, c0:c0 + 128], lhsT=ident_b[:], rhs=dmask, start=False, stop=True)
                            if c0 + 128 < 512:
                                ins = e.matmul(Sb[:, c0 + 128:512], lhsT=Km[lo:hi, j * 128:(j + 1) * 128],
                                               rhs=Qm[lo:hi, q0 + c0 + 128:q0 + 512], start=True, stop=True, skip_group_check=True)
                            return ins
                        op("pe", f, reads=rk + ["ident_b", "fmask", "dbias"], writes=[("pb", bank)])

                def emit_rest(u, uidx):
                    qt, m, js = u
                    half, Qm, Km, lo, hi, dlo, dhi, dmask, Vm, rk = m
                    slot = uidx % NSLOT_
                    Pt = PT[uidx % 4]
                    oset = state["oset"]
                    okeys = okeys_of(oset)
                    state["ui"] = uidx
                    if len(js) == 2:
                        op("act", lambda e: e.activation(out=Pt[:, 0:1024], in_=pbig[:, 2 * slot * 512:2 * slot * 512 + 1024], func=AF.Exp),
                           reads=[("pb", 2 * slot), ("pb", 2 * slot + 1)], writes=[("PT", uidx % 4)])
                    else:
                        c0_ = max(js[0] - 4 * qt, 0) * 128
                        bank1 = (2 * slot) if is_fox else slot
                        op("act", lambda e: e.activation(out=Pt[:, c0_:512], in_=pb[bank1][:, c0_:512], func=AF.Exp),
                           reads=[("pb", bank1)], writes=[("PT", uidx % 4)])
                    for k, j in enumerate(js):
                        jb = j - 4 * qt

                        def pv(e, k=k, j=j, jb=jb):
                            for qb in range(max(jb, 0), 4):
                                ins = e.matmul(Oq_ap(oset, qb), lhsT=Pt[:, k * 512 + qb * 128:k * 512 + (qb + 1) * 128],
                                               rhs=Vm[:, j, :], start=False, stop=(j == 4 * qt + qb), skip_group_check=True)
                            return ins
                        op("pe", pv, reads=[("PT", uidx % 4), "V"], writes=okeys)
                    if js[-1] == 4 * qt + 3:
                        finalize(qt, m, oset, okeys)
                        state["oset"] = 1 - oset

                def finalize(qt, m, oset, okeys):
                    half = m[0]
                    rec = small[:, 24:28]
                    for qb in range(4):
                        Oq = Oq_ap(oset, qb)
                        tile = 4 * qt + qb
                        op("dve", lambda e, Oq=Oq, qb=qb: e.reciprocal(out=rec[:, qb:qb + 1], in_=Oq[:, DV:DV + 1]),
                           reads=okeys, writes=[("rec", qb)])
                        if is_fox:
                            col = (2 * pair + half) * 64
                            op("dve", lambda e, Oq=Oq, qb=qb, tile=tile, col=col: e.tensor_scalar(
                                out=mix[:, tile, col:col + 64], in0=Oq[:, 0:64], scalar1=rec[:, qb:qb + 1], scalar2=None,
                                op0=ALU.mult), reads=okeys + [("rec", qb)], writes=[("mix", tile)])
                        elif half == 0:
                            op("dve", lambda e, Oq=Oq, qb=qb: e.tensor_scalar(
                                out=O1n[:, qb, :], in0=Oq[:, 0:128], scalar1=rec[:, qb:qb + 1], scalar2=None, op0=ALU.mult),
                                reads=okeys + [("rec", qb)], writes=[("O1n", qb)])
                        else:
                            op("dve", lambda e, qb=qb: e.tensor_tensor(out=rec[:, qb:qb + 1], in0=rec[:, qb:qb + 1], in1=nlam[:],
                                                                        op=ALU.mult), reads=[("rec", qb), "nlam"], writes=[("rec", qb)])
                            op("dve", lambda e, Oq=Oq, qb=qb: e.scalar_tensor_tensor(
                                out=dtmp[:, qb, :], in0=Oq[:, 0:128], scalar=rec[:, qb:qb + 1], in1=O1n[:, qb, :],
                                op0=ALU.mult, op1=ALU.add), reads=okeys + [("rec", qb), ("O1n", qb)], writes=[("dtmp", qb)])
                            op("dve", lambda e, qb=qb: e.scalar_tensor_tensor(
                                out=junkd[:], in0=dtmp[:, qb, :], scalar=1.0, in1=dtmp[:, qb, :], op0=ALU.mult,
                                op1=ALU.mult, accum_out=small[:, 28 + qb:29 + qb]), reads=[("dtmp", qb)], writes=["junkd", ("ssq", qb)])
                    zero_oset(okeys)
                    if (not is_fox) and half == 1:
                        def partB(qt=qt):
                            op("act", lambda e: e.activation(out=small[:, 32:36], in_=small[:, 28:32], func=AF.Ln, scale=1.0 / 128,
                                                             bias=1e-5), reads=[("ssq", q) for q in range(4)], writes=["lnss"])
                            op("act", lambda e: e.activation(out=small[:, 36:40], in_=small[:, 32:36], func=AF.Exp, scale=-0.5),
                               reads=["lnss"], writes=["rstd4"])
                            for qb in range(4):
                                tile = 4 * qt + qb
                                col = 512 + hd * 128
                                op("dve", lambda e, qb=qb, tile=tile, col=col: e.scalar_tensor_tensor(
                                    out=mix[:, tile, col:col + 128], in0=dtmp[:, qb, :], scalar=small[:, 36 + qb:37 + qb], in1=subgs[:],
                                    op0=ALU.mult, op1=ALU.mult), reads=[("dtmp", qb), "rstd4", "subgs"], writes=[("mix", tile)])
                        pending.append((state["ui"] + 4, partB))

                for os_ in range(2):
                    zero_oset(okeys_of(os_))
                for k0 in range(min(LAU_, len(units))):
                    emit_qk(units[k0], k0)
                for ui in range(len(units)):
                    if ui + LAU_ < len(units):
                        emit_qk(units[ui + LAU_], ui + LAU_)
                    emit_rest(units[ui], ui)
                    while pending and pending[0][0] <= ui:
                        pending.pop(0)[1]()
                while pending:
                    pending.pop(0)[1]()

            for pair_ in range(8):
                do_pair(pair_)

            if DEBUG:
                for i in range(NT):
                    tmpf = O1n[:, :, :].rearrange("p a b -> p (a b)")
                    for hh in range(2):
                        op("dve", lambda e, i=i, hh=hh: e.tensor_copy(out=tmpf, in_=mix[:, i, hh * 512:(hh + 1) * 512]),
                           reads=[("mix", i)], writes=["dbgt"])
                        dma("sp", lambda e, i=i, hh=hh: e.dma_start(out=dbg["mix"][i * 128:(i + 1) * 128, hh * 512:(hh + 1) * 512],
                                                                   in_=tmpf), reads=["dbgt"], is_out=True)

            if STOP == 2:
                raise _Stop()
            sc.global_barrier()
            wo = carve(0, [128, 8, D], BF16)
            wr = carve(16384, [128, 8, 36], F32)
            br = carve(17536, [128, 36], F32)
            mixT = [carve(17680 + 2048 * i, [128, 8, 128], BF16) for i in range(2)]
            xt2 = [carve(21776 + 4096 * i, [128, D], F32) for i in range(4)]
            h2T = [carve(38160 + 4096 * i, [128, 8, 128], F32) for i in range(2)]
            jb2 = carve(46352, [128, D], BF16)
            x1t = [carve(131072 + 4096 * i, [128, D], F32) for i in range(3)]
            h2f = [carve(143360 + 4096 * i, [128, D], F32) for i in range(3)]
            dma("pool", lambda e: e.dma_start(out=wo[:], in_=w_o.rearrange("(c p) n -> p c n", p=128)), writes=["wo"])
            dma("sp", lambda e: e.dma_start(out=wr[:], in_=w_r.rearrange("(c p) n -> p c n", p=128)), writes=["wr"])
            dma("sp", lambda e: e.dma_start(out=br[:], in_=b_r), writes=["br"])
            x1v = x1s.rearrange("(t p) d -> t p d", p=128)

            def d0(i):
                tpm = pb[0][:, :].bitcast(BF16)

                def trm(e, i=i, tpm=tpm):
                    for c in range(8):
                        ins = e.transpose(tpm[:, c * 128:(c + 1) * 128], mix[:, i, c * 128:(c + 1) * 128], ident_b[:])
                    return ins
                op("pe", trm, reads=[("mix", i), "ident_b"], writes=[("pb", 0)])
                dma("sp", lambda e, i=i: e.dma_start(out=xt2[i % 4][:], in_=xv[i]), writes=[("xt2", i % 4)])

            def d1(i):
                b2 = i % 2
                tpm = pb[0][:, :].bitcast(BF16)
                op("act", lambda e, tpm=tpm, b2=b2: e.copy(out=mixT[b2][:], in_=tpm.rearrange("p (c t) -> p c t", c=8)),
                   reads=[("pb", 0)], writes=[("mixT", b2)])

            def ybanks(i):
                return (1, 2) if i % 2 == 0 else (3, 4)

            def d2(i):
                b2 = i % 2
                for n in range(2):
                    bk = ybanks(i)[n]

                    def mmo(e, n=n, bk=bk, b2=b2):
                        for c in range(8):
                            ins = e.matmul(pb[bk][:], lhsT=mixT[b2][:, c, :], rhs=wo[:, c, n * 512:(n + 1) * 512],
                                           start=(c == 0), stop=(c == 7))
                        return ins
                    op("pe", mmo, reads=[("mixT", b2), "wo"], writes=[("pb", bk)])

            def d3(i):
                b3 = i % 3
                for n in range(2):
                    bk = ybanks(i)[n]
                    op("dve", lambda e, n=n, bk=bk, b3=b3: e.tensor_tensor(out=x1t[b3][:, n * 512:(n + 1) * 512], in0=pb[bk][:],
                                                                           in1=GATE1[:, n * 512:(n + 1) * 512], op=ALU.mult),
                       reads=[("pb", bk), "modr"], writes=[("x1t", b3)])
                op("dve", lambda e, i=i, b3=b3: e.tensor_tensor(out=x1t[b3][:], in0=x1t[b3][:], in1=xt2[i % 4][:], op=ALU.add),
                   reads=[("x1t", b3), ("xt2", i % 4)], writes=[("x1t", b3)])

            def d4(i):
                b2, b3 = i % 2, i % 3
                dma("sp", lambda e, i=i, b3=b3: e.dma_start(out=x1v[i], in_=x1t[b3][:]), reads=[("x1t", b3)], writes=[("x1s", i)])
                if DEBUG:
                    dma("sp", lambda e, i=i, b3=b3: e.dma_start(out=dbg["x1"][i * 128:(i + 1) * 128, :], in_=x1t[b3][:]),
                        reads=[("x1t", b3)], is_out=True)
                sq = small[:, 40 + b2 * 4: 44 + b2 * 4]
                ks = ("sq2", b2)
                op("act", lambda e, b3=b3, sq=sq: e.activation(out=jb2[:], in_=x1t[b3][:], func=AF.Square, accum_out=sq[:, 0:1]),
                   reads=[("x1t", b3)], writes=["jb2", ks])
                op("act", lambda e, sq=sq: e.activation(out=sq[:, 1:2], in_=sq[:, 0:1], func=AF.Ln, scale=1.0 / D, bias=1e-6),
                   reads=[ks], writes=[ks])
                op("act", lambda e, sq=sq: e.activation(out=sq[:, 2:3], in_=sq[:, 1:2], func=AF.Exp, scale=-0.5),
                   reads=[ks], writes=[ks])

            def d5(i):
                b2, b3 = i % 2, i % 3
                sq = small[:, 40 + b2 * 4: 44 + b2 * 4]
                ks = ("sq2", b2)
                op("dve", lambda e, b3=b3, sq=sq: e.scalar_tensor_tensor(out=h2f[b3][:], in0=x1t[b3][:], scalar=sq[:, 2:3],
                                                                          in1=A2, op0=ALU.mult, op1=ALU.mult),
                   reads=[("x1t", b3), ks, "A2"], writes=[("h2f", b3)])

            def d6(i):
                b3 = i % 3
                op("pool", lambda e, b3=b3: e.tensor_tensor(out=h2f[b3][:], in0=h2f[b3][:], in1=SH2, op=ALU.add),
                   reads=[("h2f", b3), "modr"], writes=[("h2f", b3)])

            def d7(i):
                b3 = i % 3
                op("act", lambda e, i=i, b3=b3: e.copy(out=mix[:, i, :], in_=h2f[b3][:]), reads=[("h2f", b3)], writes=[("mix", i)])
                for hh in range(2):
                    tb = pb[5 + hh]

                    def trh(e, hh=hh, tb=tb, b3=b3):
                        for c in range(4):
                            cc = hh * 4 + c
                            ins = e.transpose(tb[:, c * 128:(c + 1) * 128], h2f[b3][:, cc * 128:(cc + 1) * 128], ident_f[:])
                        return ins
                    op("pe", trh, reads=[("h2f", b3), "ident_f"], writes=[("pb", 5 + hh)])

            def d8(i):
                b2 = i % 2
                op("dve", lambda e, b2=b2: e.tensor_copy(out=h2T[b2][:, 0:4, :], in_=pb[5][:, :].rearrange("p (c t) -> p c t", c=4)),
                   reads=[("pb", 5)], writes=[("h2T", b2, 0)])
                op("act", lambda e, b2=b2: e.copy(out=h2T[b2][:, 4:8, :], in_=pb[6][:, :].rearrange("p (c t) -> p c t", c=4)),
                   reads=[("pb", 6)], writes=[("h2T", b2, 1)])

            def d9(i):
                b2 = i % 2

                def mml(e, b2=b2):
                    for c in range(8):
                        ins = e.matmul(pb[7][:, 0:36], lhsT=h2T[b2][:, c, :], rhs=wr[:, c, :], start=(c == 0), stop=(c == 7))
                    return ins
                op("pe", mml, reads=[("h2T", b2, 0), ("h2T", b2, 1), "wr"], writes=[("pb", 7)])

            def d10(i):
                op("dve", lambda e, i=i: e.tensor_tensor(out=L[:, i, :], in0=pb[7][:, 0:36], in1=br[:], op=ALU.add),
                   reads=[("pb", 7), "br"], writes=["L"])
            pipeline([d0, d1, d2, d3, d4, d5, d6, d7, d8, d9, d10], NT)

            if STOP == 3:
                raise _Stop()
            sc.global_barrier()
            R = {}
            roff = [131072]
            for nm, shp in [("gmax", [128, NT]), ("G", [128, NT, 4]), ("ge", [128, NT, 4]), ("gs", [128, NT]), ("pg", [128, NT]),
                            ("elm", [128, NT, 32]), ("v1", [128, NT]), ("M1", [128, NT, 32]), ("elm2", [128, NT, 32]),
                            ("v2", [128, NT]), ("M2", [128, NT, 32]), ("r", [128, NT]), ("w1", [128, NT]), ("w2", [128, NT]),
                            ("T", [128, NT, 32]), ("carry", [128, NT, 32]), ("pos", [128, NT, 32]), ("cnt", [128, 32]),
                            ("pad", [128, 32]), ("pends", [128, 32]), ("pst", [128, 32]), ("tmp3", [128, NT, 32]),
                            ("d1f", [128, NT]), ("d2f", [128, NT]), ("cmp", [128, NB, 32]), ("ebf", [128, NB]), ("tmpd", [128, NB]), ("skp", [128, NB]), ("ones32", [128, 32])]:
                if nm in ("w1", "w2"):
                    continue
                if nm == "cmp":
                    R[nm] = carve(0, shp, F32)
                    continue
                nbytes = 4
                for d_ in shp[1:]:
                    nbytes *= d_
                R[nm] = carve(roff[0], shp, F32)
                roff[0] += nbytes
            Mb = carve(roff[0], [128, NT, 32], BF16)
            roff[0] += 2048
            padi = carve(roff[0], [128, 32], I32)
            roff[0] += 128
            assert roff[0] <= 179200
            R["w1"] = carve(179200, [128, NT], F32)
            R["w2"] = carve(179328, [128, NT], F32)
            d1u = carve(179456, [128, NT], U32)
            d2u = carve(179584, [128, NT], U32)
            widx = carve(179712, [128, NB], U32)

            def bc_last(ap2, n):
                return ap2.unsqueeze(2).to_broadcast([128, ap2.shape[1], n])

            def dv(fn, reads, writes):
                return op("dve", fn, reads=reads, writes=writes)
            gl = L[:, :, 0:4]
            el = L[:, :, 4:36]
            dv(lambda e: e.tensor_reduce(out=R["gmax"][:], in_=gl, axis=AX.X, op=ALU.max), ["L"], ["gmax"])
            dv(lambda e: e.tensor_tensor(out=R["G"][:], in0=gl, in1=bc_last(R["gmax"][:], 4), op=ALU.is_equal), ["L", "gmax"], ["G"])
            dv(lambda e: e.tensor_tensor(out=R["ge"][:], in0=gl, in1=bc_last(R["gmax"][:], 4), op=ALU.subtract), ["L", "gmax"], ["ge"])
            op("act", lambda e: e.activation(out=R["ge"][:], in_=R["ge"][:], func=AF.Exp), reads=["ge"], writes=["ge"])
            dv(lambda e: e.tensor_reduce(out=R["gs"][:], in_=R["ge"][:], axis=AX.X, op=ALU.add), ["ge"], ["gs"])
            dv(lambda e: e.reciprocal(out=R["pg"][:], in_=R["gs"][:]), ["gs"], ["pg"])
            dv(lambda e: e.tensor_scalar(out=R["G"][:], in0=R["G"][:], scalar1=-1.0, scalar2=1e4, op0=ALU.add, op1=ALU.mult),
               ["G"], ["G"])
            for g in range(4):
                dv(lambda e, g=g: e.tensor_tensor(out=R["elm"][:, :, g * 8:(g + 1) * 8], in0=el[:, :, g * 8:(g + 1) * 8],
                                                  in1=bc_last(R["G"][:, :, g], 8), op=ALU.add), ["L", "G"], ["elm"])
            dv(lambda e: e.tensor_reduce(out=R["v1"][:], in_=R["elm"][:], axis=AX.X, op=ALU.max), ["elm"], ["v1"])
            dv(lambda e: e.tensor_tensor(out=R["M1"][:], in0=R["elm"][:], in1=bc_last(R["v1"][:], 32), op=ALU.is_equal),
               ["elm", "v1"], ["M1"])
            dv(lambda e: e.scalar_tensor_tensor(out=R["elm2"][:], in0=R["M1"][:], scalar=-1e4, in1=R["elm"][:], op0=ALU.mult,
                                                op1=ALU.add), ["M1", "elm"], ["elm2"])
            dv(lambda e: e.tensor_reduce(out=R["v2"][:], in_=R["elm2"][:], axis=AX.X, op=ALU.max), ["elm2"], ["v2"])
            dv(lambda e: e.tensor_tensor(out=R["M2"][:], in0=R["elm2"][:], in1=bc_last(R["v2"][:], 32), op=ALU.is_equal),
               ["elm2", "v2"], ["M2"])
            dv(lambda e: e.tensor_tensor(out=R["r"][:], in0=R["v2"][:], in1=R["v1"][:], op=ALU.subtract), ["v1", "v2"], ["r"])
            op("act", lambda e: e.activation(out=R["r"][:], in_=R["r"][:], func=AF.Exp), reads=["r"], writes=["r"])
            dv(lambda e: e.tensor_scalar(out=R["w1"][:], in0=R["r"][:], scalar1=1.0, scalar2=None, op0=ALU.add), ["r"], ["w1"])
            dv(lambda e: e.reciprocal(out=R["w1"][:], in_=R["w1"][:]), ["w1"], ["w1"])
            dv(lambda e: e.tensor_tensor(out=R["w1"][:], in0=R["w1"][:], in1=R["pg"][:], op=ALU.mult), ["w1", "pg"], ["w1"])
            dv(lambda e: e.tensor_tensor(out=R["w2"][:], in0=R["w1"][:], in1=R["r"][:], op=ALU.mult), ["w1", "r"], ["w2"])
            dv(lambda e: e.tensor_tensor(out=Mb[:], in0=R["M1"][:], in1=R["M2"][:], op=ALU.add), ["M1", "M2"], ["Mb"])

            def mmT(e):
                for i in range(NT):
                    ins = e.matmul(pb[i // 16][:, (i % 16) * 32:(i % 16 + 1) * 32], lhsT=ones_b[:], rhs=Mb[:, i, :],
                                   start=True, stop=True, skip_group_check=True)
                return ins
            op("pe", mmT, reads=["ones_b", "Mb"], writes=[("pb", 0), ("pb", 1)])
            for hh in range(2):
                dv(lambda e, hh=hh: e.tensor_copy(out=R["T"][:, hh * 16:(hh + 1) * 16, :],
                                                  in_=pb[hh][:, :].rearrange("p (t k) -> p t k", t=16)), [("pb", hh)], ["T"])

            def mmP(e):
                for i in range(NT):
                    ins = e.matmul(pb[2 + i // 16][:, (i % 16) * 32:(i % 16 + 1) * 32], lhsT=stri[:], rhs=Mb[:, i, :],
                                   start=True, stop=True, skip_group_check=True)
                return ins
            op("pe", mmP, reads=["stri", "Mb"], writes=[("pb", 2), ("pb", 3)])
            Tem, Cem, rmask = R["elm"], R["elm2"], R["tmp3"]
            flat = lambda ap3: ap3[:, :, :].rearrange("p a b -> p (a b)")
            dv(lambda e: e.tensor_copy(out=Tem[:, :, :], in_=R["T"][:, :, :].rearrange("p t k -> p k t")), ["T", "M1", "M2"], ["elm"])
            dv(lambda e: e.memset(rmask[:, :, :], 1.0), [], ["tmp3"])
            dv(lambda e: e.memset(rmask[:, :, 0:1], 0.0), ["tmp3"], ["tmp3"])
            dv(lambda e: e.tensor_tensor_scan(out=flat(Cem), data0=flat(rmask), data1=flat(Tem), initial=0.0,
                                              op0=ALU.mult, op1=ALU.add), ["elm", "tmp3", "M2", "v2"], ["elm2"])
            dv(lambda e: e.tensor_copy(out=R["cnt"][:], in_=Cem[:, :, NT - 1]), ["elm2"], ["cnt"])
            dv(lambda e: e.tensor_tensor(out=Tem[:, :, :], in0=Cem[:, :, :], in1=Tem[:, :, :], op=ALU.subtract), ["elm2", "elm"], ["elm"])
            dv(lambda e: e.tensor_copy(out=R["carry"][:, :, :], in_=Tem[:, :, :].rearrange("p k t -> p t k")), ["elm"], ["carry"])
            for hh in range(2):
                dv(lambda e, hh=hh: e.tensor_tensor(out=R["pos"][:, hh * 16:(hh + 1) * 16, :],
                                                    in0=pb[2 + hh][:, :].rearrange("p (t k) -> p t k", t=16),
                                                    in1=R["carry"][:, hh * 16:(hh + 1) * 16, :], op=ALU.add),
                   [("pb", 2 + hh), "carry"], ["pos"])
            dv(lambda e: e.tensor_scalar(out=padi[:], in0=R["cnt"][:], scalar1=float(BLK - 1), scalar2=None, op0=ALU.add),
               ["cnt"], ["padi"])
            dv(lambda e: e.tensor_scalar(out=padi[:], in0=padi[:], scalar1=7, scalar2=7, op0=ALU.arith_shift_right,
                                         op1=ALU.logical_shift_left), ["padi"], ["padi"])
            dv(lambda e: e.tensor_copy(out=R["pad"][:], in_=padi[:]), ["padi"], ["pad"])
            dv(lambda e: e.memset(R["ones32"][:], 1.0), [], ["ones32"])
            dv(lambda e: e.tensor_tensor_scan(out=R["pends"][:], data0=R["ones32"][:], data1=R["pad"][:], initial=0.0,
                                              op0=ALU.mult, op1=ALU.add), ["pad", "ones32"], ["pends"])
            dv(lambda e: e.tensor_tensor(out=R["pst"][:], in0=R["pends"][:], in1=R["pad"][:], op=ALU.subtract),
               ["pends", "pad"], ["pst"])
            dv(lambda e: e.tensor_tensor(out=R["pos"][:], in0=R["pos"][:], in1=R["pst"][:].unsqueeze(1).to_broadcast([128, NT, 32]),
                                         op=ALU.add), ["pos", "pst"], ["pos"])
            for Mk, dk, du in (("M1", "d1f", d1u), ("M2", "d2f", d2u)):
                dv(lambda e, Mk=Mk: e.tensor_tensor(out=R["tmp3"][:], in0=R[Mk][:], in1=R["pos"][:], op=ALU.mult),
                   [Mk, "pos"], ["tmp3"])
                dv(lambda e, dk=dk: e.tensor_reduce(out=R[dk][:], in_=R["tmp3"][:], axis=AX.X, op=ALU.add), ["tmp3"], [dk])
                dv(lambda e, dk=dk, du=du: e.tensor_copy(out=du[:], in_=R[dk][:]), [dk], [dk + "u"])
            dv(lambda e: e.tensor_tensor(out=R["cmp"][:], in0=R["pends"][:].unsqueeze(1).to_broadcast([128, NB, 32]),
                                         in1=bc_last(jgrid[:], 32), op=ALU.is_le), ["pends", "jgrid"], ["cmp"])
            dv(lambda e: e.tensor_reduce(out=R["ebf"][:], in_=R["cmp"][:], axis=AX.X, op=ALU.add), ["cmp"], ["ebf"])
            dv(lambda e: e.memset(R["skp"][:], 0.0), [], ["skp"])
            dv(lambda e: e.tensor_tensor(out=R["skp"][:, 1:NB], in0=R["ebf"][:, 1:NB], in1=R["ebf"][:, 0:NB - 1], op=ALU.is_equal),
               ["ebf", "skp"], ["skp"])
            for st_ in range(1, NSTR):
                c0_ = st_ * (NB // NSTR)
                dv(lambda e, c0_=c0_: e.memset(R["skp"][:, c0_:c0_ + 1], 0.0), ["skp"], ["skp"])
            dv(lambda e: e.tensor_scalar(out=R["skp"][:], in0=R["skp"][:], scalar1=1.0e6, scalar2=pcol[:, 0:1], op0=ALU.mult,
                                         op1=ALU.add), ["skp", "pcol"], ["skp"])
            dv(lambda e: e.scalar_tensor_tensor(out=R["tmpd"][:], in0=R["ebf"][:], scalar=128.0, in1=R["skp"][:], op0=ALU.mult,
                                                op1=ALU.add), ["ebf", "skp"], ["tmpd"])
            dv(lambda e: e.tensor_copy(out=widx[:], in_=R["tmpd"][:]), ["tmpd"], ["widx"])
            if DEBUG:
                rt = carve(180224, [128, NT, 4], F32)
                dv(lambda e: e.tensor_copy(out=rt[:, :, 0], in_=R["d1f"][:]), ["d1f"], ["rt"])
                dv(lambda e: e.tensor_copy(out=rt[:, :, 1], in_=R["d2f"][:]), ["d2f"], ["rt"])
                dv(lambda e: e.tensor_copy(out=rt[:, :, 2], in_=R["w1"][:]), ["w1"], ["rt"])
                dv(lambda e: e.tensor_copy(out=rt[:, :, 3], in_=R["w2"][:]), ["w2"], ["rt"])
                dma("sp", lambda e: e.dma_start(out=dbg["rt"], in_=rt[:]), reads=["rt"], is_out=True)

            if STOP == 4:
                raise _Stop()
            for i in range(NT):
                for du, dk in ((d1u, "d1fu"), (d2u, "d2fu")):
                    dma("pool", lambda e, i=i, du=du: e.indirect_dma_start(
                        out=xs[:, :], out_offset=bass.IndirectOffsetOnAxis(ap=du[:, i:i + 1], axis=0),
                        in_=mix[:, i, :], in_offset=None), reads=[("mix", i), dk], writes=[("xs", i, dk)])

            if STOP == 5:
                raise _Stop()
            sc.global_barrier()
            wg = [carve(0 + 8192 * i, [128, 8, 512], BF16) for i in range(NSTR)]
            wu = [carve(32768 + 8192 * i, [128, 8, 512], BF16) for i in range(NSTR)]
            wd = [carve(65536 + 8192 * i, [128, 4, D], BF16) for i in range(NSTR)]
            xb = [carve(98304 + 2048 * i, [128, D], BF16) for i in range(2)]
            xT = [carve(102400 + 2048 * i, [128, 8, BLK], BF16) for i in range(2)]
            hidT = [carve(106496 + 1024 * i, [128, 4, BLK], BF16) for i in range(2)]
            sg = [carve(108544 + 512 * i, [128, BLK], F32) for i in range(2)]
            yo = [carve(131072 + 2048 * i, [128, D], BF16) for i in range(2)]
            xsv = xs.rearrange("(j p) d -> j p d", p=128)
            ysv = ys.rearrange("(j p) d -> j p d", p=128)
            order = [k + (NB // NSTR) * st_ for k in range(NB // NSTR) for st_ in range(NSTR)]
            bregs = {}
            sc.streams["pool"].append(lambda e: bregs.update(g=e.to_reg(32 * 128 - 1)))

            def gA(pi):
                j, ws, b2 = order[pi], pi % NSTR, pi % 2
                for (wsb, wdr, nm) in ((wg[ws], wgb, "wg"), (wu[ws], wub, "wu"), (wd[ws], wdb, "wd")):
                    dma("pool", lambda e, wsb=wsb, wdr=wdr, j=j: e.indirect_dma_start(
                        out=wsb[:, :, :].rearrange("p a b -> p (a b)"), out_offset=None, in_=wdr[:, :],
                        in_offset=bass.IndirectOffsetOnAxis(ap=widx[:, j:j + 1], axis=0),
                        bounds_check=bregs["g"], oob_is_err=False),
                        reads=["widx"], writes=[(nm, ws)])
                dma("sp", lambda e, j=j, b2=b2: e.dma_start(out=xb[b2][:], in_=xsv[j]), reads=[], writes=[("xb", b2)])

            def gA1(pi):
                j, ws, b2 = order[pi], pi % NSTR, pi % 2
                for hh in range(2):
                    bank = 0 if hh == 0 else 7
                    tb = pb[bank][:, :].bitcast(BF16)[:, 0:512]

                    def trx(e, hh=hh, tb=tb, b2=b2):
                        for c in range(4):
                            cc = hh * 4 + c
                            ins = e.transpose(tb[:, c * 128:(c + 1) * 128], xb[b2][:, cc * 128:(cc + 1) * 128], ident_b[:])
                        return ins
                    op("pe", trx, reads=[("xb", b2), "ident_b"], writes=[("pb", bank)])
                    if hh == 0:
                        op("act", lambda e, hh=hh, tb=tb, b2=b2: e.copy(
                            out=xT[b2][:, hh * 4:(hh + 1) * 4, :], in_=tb.rearrange("p (c t) -> p c t", c=4)),
                           reads=[("pb", bank)], writes=[("xT", b2, hh)])
                    else:
                        op("dve", lambda e, hh=hh, tb=tb, b2=b2: e.tensor_copy(
                            out=xT[b2][:, hh * 4:(hh + 1) * 4, :], in_=tb.rearrange("p (c t) -> p c t", c=4)),
                           reads=[("pb", bank)], writes=[("xT", b2, hh)])

            def gB(pi):
                j, ws, b2 = order[pi], pi % NSTR, pi % 2
                xtk = [("xT", b2, 0), ("xT", b2, 1)]
                for fc in range(4):
                    pg_, pu_ = pb[1 + fc % 2][:, 0:BLK], pb[1 + fc % 2][:, BLK:2 * BLK]
                    kg = ("pb", 1 + fc % 2)

                    def mmg(e, fc=fc, pg_=pg_, ws=ws, b2=b2):
                        for c in range(8):
                            ins = e.matmul(pg_, lhsT=wg[ws][:, c, fc * 128:(fc + 1) * 128], rhs=xT[b2][:, c, :],
                                           start=(c == 0), stop=(c == 7))
                        return ins

                    def mmu(e, fc=fc, pu_=pu_, ws=ws, b2=b2):
                        for c in range(8):
                            ins = e.matmul(pu_, lhsT=wu[ws][:, c, fc * 128:(fc + 1) * 128], rhs=xT[b2][:, c, :],
                                           start=(c == 0), stop=(c == 7))
                        return ins
                    op("pe", mmg, reads=[("wg", ws)] + xtk, writes=[kg])
                    op("pe", mmu, reads=[("wu", ws)] + xtk, writes=[kg])
                    op("act", lambda e, fc=fc, pg_=pg_: e.activation(out=sg[fc % 2][:], in_=pg_, func=AF.Silu),
                       reads=[kg], writes=[("sg", fc % 2)])
                    op("dve", lambda e, fc=fc, pu_=pu_, b2=b2: e.tensor_tensor(out=hidT[b2][:, fc, :], in0=sg[fc % 2][:],
                                                                               in1=pu_, op=ALU.mult),
                       reads=[("sg", fc % 2), kg], writes=[("hidT", b2, fc)])

            def gC(pi):
                j, ws, b2 = order[pi], pi % NSTR, pi % 2
                for n in range(2):
                    ybk = 3 + 2 * b2 + n
                    yb = pb[ybk]

                    def mmd(e, n=n, yb=yb, ws=ws, b2=b2):
                        for fc in range(4):
                            ins = e.matmul(yb[:], lhsT=hidT[b2][:, fc, :], rhs=wd[ws][:, fc, n * 512:(n + 1) * 512],
                                           start=(fc == 0), stop=(fc == 3))
                        return ins
                    op("pe", mmd, reads=[("hidT", b2, f) for f in range(4)] + [("wd", ws)], writes=[("pb", ybk)])
                    if n == 0:
                        op("act", lambda e, n=n, yb=yb, b2=b2: e.copy(out=yo[b2][:, n * 512:(n + 1) * 512], in_=yb[:]),
                           reads=[("pb", ybk)], writes=[("yo", b2, n)])
                    else:
                        op("dve", lambda e, n=n, yb=yb, b2=b2: e.tensor_copy(out=yo[b2][:, n * 512:(n + 1) * 512], in_=yb[:]),
                           reads=[("pb", ybk)], writes=[("yo", b2, n)])
                dma("sp", lambda e, j=j, b2=b2: e.dma_start(out=ysv[j], in_=yo[b2][:]),
                    reads=[("yo", b2, 0), ("yo", b2, 1)], writes=[("ys", j)])
            pipeline([gA, gA1, gB, gC], NB)

            if STOP == 6:
                raise _Stop()
            y1 = [carve(139264 + 2048 * i, [128, D], BF16) for i in range(2)]
            y2 = [carve(143360 + 2048 * i, [128, D], BF16) for i in range(3)]
            mo = [carve(149504 + 4096 * i, [128, D], F32) for i in range(3)]
            x1t_h = [carve(161792 + 4096 * i, [128, D], F32) for i in range(3)]
            h2f_h = [carve(174080, [128, D], F32), carve(126976, [128, D], F32)]
            jb2_h = carve(120832, [128, D], BF16)
            gf = carve(122880, [128, D], F32)
            dma("sp", lambda e: e.dma_start(out=gf, in_=gvec[:, 2, :]), writes=["gf"])
            ov = out.rearrange("(t p) d -> t p d", p=128)

            def h0(i):
                dma("pool", lambda e, i=i: e.indirect_dma_start(
                    out=y1[i % 2][:, :], out_offset=None, in_=ys[:, :],
                    in_offset=bass.IndirectOffsetOnAxis(ap=d1u[:, i:i + 1], axis=0)), reads=[("ys", j_) for j_ in range(NB)] + ["d1fu"], writes=[("y1", i % 2)])
                dma("pool", lambda e, i=i: e.indirect_dma_start(
                    out=y2[i % 3][:, :], out_offset=None, in_=ys[:, :],
                    in_offset=bass.IndirectOffsetOnAxis(ap=d2u[:, i:i + 1], axis=0)), reads=[("ys", j_) for j_ in range(NB)] + ["d2fu"], writes=[("y2", i % 3)])

            def h1(i):
                op("act", lambda e, i=i: e.activation(out=mo[i % 3][:], in_=y1[i % 2][:], func=AF.Copy, scale=R["w1"][:, i:i + 1]),
                   reads=[("y1", i % 2), "w1"], writes=[("mo", i % 3)])

            def h2(i):
                op("dve", lambda e, i=i: e.scalar_tensor_tensor(out=mo[i % 3][:], in0=y2[i % 3][:], scalar=R["w2"][:, i:i + 1],
                                                               in1=mo[i % 3][:], op0=ALU.mult, op1=ALU.add),
                   reads=[("y2", i % 3), "w2", ("mo", i % 3)], writes=[("mo", i % 3)])

            def h3(i):
                op("dve", lambda e, i=i: e.tensor_tensor(out=mo[i % 3][:], in0=mo[i % 3][:], in1=GATE2, op=ALU.mult),
                   reads=[("mo", i % 3), "modr"], writes=[("mo", i % 3)])
                dma("sp", lambda e, i=i: e.dma_start(out=x1t_h[i % 3][:], in_=x1v[i]), reads=[("x1s", i)], writes=[("x1t_h", i % 3)])

            def h4(i):
                op("dve", lambda e, i=i: e.tensor_tensor(out=x1t_h[i % 3][:], in0=x1t_h[i % 3][:], in1=mo[i % 3][:], op=ALU.add),
                   reads=[("mo", i % 3), ("x1t_h", i % 3)], writes=[("x1t_h", i % 3)])

            def h5(i):
                b2 = i % 2
                sq = small[:, 48 + b2 * 4: 52 + b2 * 4]
                ks = ("sq3", b2)
                op("act", lambda e, i=i, sq=sq: e.activation(out=jb2_h[:], in_=x1t_h[i % 3][:], func=AF.Square, accum_out=sq[:, 0:1]),
                   reads=[("x1t_h", i % 3)], writes=["jb2_h", ks])
                op("act", lambda e, sq=sq: e.activation(out=sq[:, 1:2], in_=sq[:, 0:1], func=AF.Ln, scale=1.0 / D, bias=1e-6),
                   reads=[ks], writes=[ks])
                op("act", lambda e, sq=sq: e.activation(out=sq[:, 2:3], in_=sq[:, 1:2], func=AF.Exp, scale=-0.5),
                   reads=[ks], writes=[ks])

            def h6(i):
                b2 = i % 2
                sq = small[:, 48 + b2 * 4: 52 + b2 * 4]
                ks = ("sq3", b2)
                op("dve", lambda e, i=i, b2=b2, sq=sq: e.scalar_tensor_tensor(out=h2f_h[b2][:], in0=x1t_h[i % 3][:], scalar=sq[:, 2:3],
                                                                               in1=gf, op0=ALU.mult, op1=ALU.mult),
                   reads=[("x1t_h", i % 3), ks, "gf"], writes=[("h2f_h", b2)])
                dma("sp", lambda e, i=i, b2=b2: e.dma_start(out=ov[i], in_=h2f_h[b2][:]), reads=[("h2f_h", b2)], is_out=True)
            pipeline([h0, h1, h2, h3, h4, h5, h6], NT)
        except _Stop:
            pass

        block = es.enter_context(nc.Block())
        sc.finish(block)
    return nc


def _consts():
    k = np.arange(128)[:, None]
    q = np.arange(128)[None, :]
    ident = np.eye(128, dtype=np.float32)
    fmask = np.where(k <= q, 0.0, NEG).astype(np.float32)
    slopes = [2.0 ** (-8.0 * (i + 1) / 4) for i in range(4)]
    dbias = np.zeros((128, 4, 128), np.float32)
    ok = (q // 64) >= (k // 64)
    for h in range(4):
        dbias[:, h, :] = np.where(ok, -slopes[h] * np.abs(q - k), NEG)
    t = np.arange(S)
    thi = (t // 64) * 64.0
    tlo = (t % 64) * 1.0
    qaug = np.zeros((4, 4, S), np.float32)
    kaug = np.zeros((4, 4, S), np.float32)
    for h in range(4):
        qaug[h, 0] = -slopes[h] * thi
        qaug[h, 1] = -slopes[h] * tlo
        qaug[h, 2] = 1.0
        qaug[h, 3] = 1.0
        kaug[h, 0] = 1.0
        kaug[h, 1] = 1.0
        kaug[h, 2] = slopes[h] * thi
        kaug[h, 3] = slopes[h] * tlo
    stri = (k < q).astype(np.float32)
    pcol = np.arange(128, dtype=np.float32).reshape(128, 1)
    jgrid = np.tile((np.arange(NB, dtype=np.float32) * BLK)[None, :], (128, 1))
    return dict(ident=ident, fmask=fmask, dbias=dbias, qaug=qaug, kaug=kaug, stri=stri, pcol=pcol, jgrid=jgrid)


def _rep(v):
    return np.ascontiguousarray(np.broadcast_to(np.asarray(v, np.float32).reshape(1, -1), (128, v.size)))


def make_in_maps(x, c, ada_w, ada_b, norm1_g, w_in, b_f, lam_q1, lam_k1, lam_q2, lam_k2, subln_g, w_o, norm2_g,
                 w_rg, b_rg, w_re, b_re, w_gate, w_up, w_down, norm_f_g):
    f = np.float32
    consts = _consts()
    shared = dict(
        ada_w=np.ascontiguousarray(ada_w[0], f),
        adab=_rep(ada_b[0]),
        gvec=np.ascontiguousarray(np.stack([_rep(norm1_g[0]), _rep(norm2_g[0]), _rep(norm_f_g)], axis=1)),
        w_in=np.ascontiguousarray(w_in[0], f),
        lamv=np.ascontiguousarray(np.stack([_rep(lam_q1[0]), _rep(lam_k1[0]), _rep(lam_q2[0]), _rep(lam_k2[0])], axis=1)),
        subg=_rep(subln_g[0]),
        w_o=np.ascontiguousarray(w_o[0], f),
        w_r=np.ascontiguousarray(np.concatenate([w_rg[0], w_re[0]], axis=1), f),
        b_r=_rep(np.concatenate([b_rg[0], b_re[0]])),
        w_gate=np.ascontiguousarray(w_gate[0], f).reshape(32 * 1024, 512),
        w_up=np.ascontiguousarray(w_up[0], f).reshape(32 * 1024, 512),
        w_down=np.ascontiguousarray(w_down[0], f).reshape(32 * 512, 1024),
    )
    shared.update(consts)
    shared["negbf"] = np.ascontiguousarray(np.asarray(b_f[0], f).reshape(8, 1))
    maps = []
    for b in range(8):
        m = dict(shared)
        m["x"] = np.ascontiguousarray(x[b], f)
        cb = np.asarray(c[b], f).reshape(8, 128)
        m["crep"] = np.ascontiguousarray(np.broadcast_to(cb.T[:, :, None], (128, 8, 128)))
        maps.append(m)
    return maps


_NC_CACHE = {}


def kernel(**inputs):
    inputs = {k: np.asarray(v) for k, v in inputs.items()}
    if "nc" not in _NC_CACHE:
        _NC_CACHE["nc"] = build_nc()
    nc = _NC_CACHE["nc"]
    in_maps = make_in_maps(**inputs)
    res = run_bass_kernel_spmd(nc, in_maps, core_ids=list(range(8)))
    kernel.last = res
    return np.stack([np.asarray(r["out"], np.float32) for r in res.results], axis=0)
```
